# Optimizing a Trainium2 kernel written in Bass

```python
import math
import jax, jax.numpy as jnp
from jax import lax
import numpy as np

D_MODEL = 2048
BATCH = 4
SEQ = 8192
DEPTH = 1

GRID_W = 64
CTX_LEN = 256
RWKV_HEADS = 16
RWKV_HEAD_DIM = 64
RWKV_DIM = RWKV_HEADS * RWKV_HEAD_DIM
DECAY_LORA = 96
AAA_LORA = 96
GATE_LORA = 256
RWKV_COLS = 3 * RWKV_DIM + 2 * DECAY_LORA + 2 * AAA_LORA + GATE_LORA
DIFF_HEADS = 8
DIFF_QK_DIM = 64
DIFF_V_DIM = 2 * DIFF_QK_DIM
DIFF_DIM = DIFF_HEADS * DIFF_V_DIM
DIFF_QK_COLS = DIFF_HEADS * 2 * DIFF_QK_DIM
DIFF_COLS = 2 * DIFF_QK_COLS + DIFF_DIM
DIFF_SCALE = DIFF_QK_DIM ** -0.5
ROPE_THETA = 10000.0
Q_BLOCK = 128
GATE_COLS = 2 * D_MODEL
N_IN = RWKV_COLS + DIFF_COLS + GATE_COLS
N_GROUPS = 4
EXPERTS_PER_GROUP = 8
N_EXPERTS = N_GROUPS * EXPERTS_PER_GROUP
TOP_K = 2
D_EXPERT = 512
MOE_BLOCK = 128
NORM_EPS = 1e-6
SUBLN_EPS = 1e-5
LNX_EPS = 64e-5

kernel_name = "hybrid_rwkv7_diffattn_hmoe_dit"


def rmsnorm(x, g, eps=NORM_EPS):
    xf = x.astype(jnp.float32)
    y = xf * lax.rsqrt(jnp.mean(xf * xf, axis=-1, keepdims=True) + eps)
    return (y * g.astype(jnp.float32)).astype(x.dtype)


def centred_shift(p, mu):
    zero = jnp.zeros_like(p[:, :1])
    prev = jnp.concatenate([zero, p[:, :-1]], axis=1)
    nxt = jnp.concatenate([p[:, 1:], zero], axis=1)
    return p + mu * (0.5 * (prev + nxt) - p)


def rwkv7_scan(s0, r, decay, k, v, kk, a, reverse):
    def step(S, inp):
        r_t, w_t, k_t, v_t, kk_t, a_t = inp
        sa = jnp.einsum('bhvk,bhk->bhv', S, kk_t)
        S = (S * w_t[:, :, None, :] - sa[..., None] * (kk_t * a_t)[:, :, None, :]
             + v_t[..., None] * k_t[:, :, None, :])
        return S, jnp.einsum('bhvk,bhk->bhv', S, r_t)
    xs = tuple(jnp.moveaxis(t, 1, 0) for t in (r, decay, k, v, kk, a))
    s_final, y = lax.scan(step, s0, xs, reverse=reverse)
    return s_final, jnp.moveaxis(y, 0, 1)


def rwkv_branch(p, s0_f, s0_b, w0, w2, a0, a2, g2, k_k, k_a, r_k, lnx_g, lnx_b):
    B_, L, _ = p.shape
    pf = p.astype(jnp.float32)
    C = RWKV_DIM
    r, k, v = pf[..., :C], pf[..., C:2 * C], pf[..., 2 * C:3 * C]
    o = 3 * C
    xw = pf[..., o:o + 2 * DECAY_LORA].reshape(B_, L, 2, DECAY_LORA)
    o += 2 * DECAY_LORA
    xa = pf[..., o:o + 2 * AAA_LORA].reshape(B_, L, 2, AAA_LORA)
    o += 2 * AAA_LORA
    xg = pf[..., o:o + GATE_LORA]
    w_log = -jax.nn.softplus(-(w0 + jnp.einsum('bldr,drc->bldc', jnp.tanh(xw), w2))) - 0.5
    decay = jnp.exp(-jnp.exp(w_log))
    a = jax.nn.sigmoid(a0 + jnp.einsum('bldr,drc->bldc', xa, a2))
    g = jax.nn.sigmoid(xg) @ g2
    heads = lambda t: t.reshape(*t.shape[:-1], RWKV_HEADS, RWKV_HEAD_DIM)
    kk = heads(k * k_k)
    kk = kk * lax.rsqrt(jnp.sum(kk * kk, axis=-1, keepdims=True) + 1e-12)
    kd = k[:, :, None, :] * (1.0 + (a - 1.0) * k_a)
    r_h, v_h, kd_h, a_h, dec_h = heads(r), heads(v), heads(kd), heads(a), heads(decay)
    s_f, y_f = rwkv7_scan(s0_f, r_h, dec_h[:, :, 0], kd_h[:, :, 0], v_h, kk, a_h[:, :, 0], reverse=False)
    s_b, y_b = rwkv7_scan(s0_b, r_h, dec_h[:, :, 1], kd_h[:, :, 1], v_h, kk, a_h[:, :, 1], reverse=True)
    y = y_f + y_b
    mean = jnp.mean(y, axis=-1, keepdims=True)
    var = jnp.mean(jnp.square(y - mean), axis=-1, keepdims=True)
    y = (y - mean) * lax.rsqrt(var + LNX_EPS) * heads(lnx_g) + heads(lnx_b)
    coef = jnp.einsum('blhn,bldhn,hn->blh', r_h, kd_h, r_k)
    y = y + coef[..., None] * v_h
    out = (y.reshape(B_, L, C) * g).astype(p.dtype)
    return out, s_f, s_b


def diff_qkv(p, qn_g, kn_g):
    B_, L, _ = p.shape
    q = p[..., :DIFF_QK_COLS].reshape(B_, L, DIFF_HEADS, 2, DIFF_QK_DIM)
    k = p[..., DIFF_QK_COLS:2 * DIFF_QK_COLS].reshape(B_, L, DIFF_HEADS, 2, DIFF_QK_DIM)
    v = p[..., 2 * DIFF_QK_COLS:].reshape(B_, L, DIFF_HEADS, DIFF_V_DIM)
    return rmsnorm(q, qn_g), rmsnorm(k, kn_g), v


def axial_rope(x, rows, cols):
    half = DIFF_QK_DIM // 2
    inv_freq = ROPE_THETA ** (-jnp.arange(0, half, 2, dtype=jnp.float32) / half)
    def rot(xs, pos):
        ang = pos.astype(jnp.float32)[:, None] * inv_freq
        cos = jnp.cos(ang)[None, :, None, None, :]
        sin = jnp.sin(ang)[None, :, None, None, :]
        x1, x2 = jnp.split(xs.astype(jnp.float32), 2, axis=-1)
        return jnp.concatenate([x1 * cos - x2 * sin, x2 * cos + x1 * sin], axis=-1)
    out = jnp.concatenate([rot(x[..., :half], rows), rot(x[..., half:], cols)], axis=-1)
    return out.astype(x.dtype)


def diff_attn_block(q, k, v, lam):
    s = jnp.einsum('bqhmd,bkhmd->bmhqk', q, k).astype(jnp.float32) * DIFF_SCALE
    p = jax.nn.softmax(s, axis=-1)
    a = p[:, 0] - lam * p[:, 1]
    return jnp.einsum('bhqk,bkhd->bqhd', a.astype(v.dtype), v)


def diff_attn_latent(q, k_all, v_all, lam):
    B_, L = q.shape[:2]
    nb = L // Q_BLOCK
    qb = jnp.moveaxis(q.reshape(B_, nb, Q_BLOCK, *q.shape[2:]), 1, 0)
    o = lax.map(lambda qi: diff_attn_block(qi, k_all, v_all, lam), qb)
    return jnp.moveaxis(o, 0, 1).reshape(B_, L, *o.shape[3:])


def diff_out(o, subln_g, lam_init):
    B_, L = o.shape[:2]
    return (rmsnorm(o, subln_g, SUBLN_EPS) * (1.0 - lam_init)).reshape(B_, L, DIFF_DIM)


def merge_branches(p_gate, y_rwkv, y_diff, w_pa, w_pb, w_out):
    gates = jax.nn.sigmoid(p_gate.reshape(*p_gate.shape[:-1], 2, D_MODEL))
    mixed = gates[..., 0, :] * (y_rwkv @ w_pa) + gates[..., 1, :] * (y_diff @ w_pb)
    return mixed @ w_out


def hierarchical_moe(h, wg, bg, we, be, w1, w3, w2):
    B_, L, D = h.shape
    hf = h.reshape(-1, D)
    N = hf.shape[0]
    g_logits = (hf @ wg + bg).astype(jnp.float32)
    p_group = jax.nn.softmax(g_logits, axis=-1)
    g_top = jnp.argmax(g_logits, axis=-1)
    p_g = jnp.take_along_axis(p_group, g_top[:, None], axis=1)[:, 0]
    e_logits = (hf @ we + be).astype(jnp.float32).reshape(N, N_GROUPS, EXPERTS_PER_GROUP)
    e_sel = jnp.take_along_axis(e_logits, g_top[:, None, None], axis=1)[:, 0]
    top_p, top_i = lax.top_k(jax.nn.softmax(e_sel, axis=-1), TOP_K)
    top_p = top_p / jnp.sum(top_p, axis=-1, keepdims=True)
    gate = p_g[:, None] * top_p
    expert = g_top[:, None] * EXPERTS_PER_GROUP + top_i
    A = N * TOP_K
    flat_e = expert.reshape(-1).astype(jnp.int32)
    flat_tok = jnp.repeat(jnp.arange(N, dtype=jnp.int32), TOP_K)
    flat_w = gate.reshape(-1)
    order = jnp.argsort(flat_e)
    se = flat_e[order]
    counts = jnp.bincount(flat_e, length=N_EXPERTS)
    padded = (counts + MOE_BLOCK - 1) // MOE_BLOCK * MOE_BLOCK
    pend = jnp.cumsum(padded)
    pstart = pend - padded
    start = jnp.cumsum(counts) - counts
    slot = pstart[se] + jnp.arange(A, dtype=jnp.int32) - start[se]
    CAP = (A + MOE_BLOCK - 1) // MOE_BLOCK * MOE_BLOCK + N_EXPERTS * MOE_BLOCK
    n_blocks = CAP // MOE_BLOCK
    slot_tok = jnp.full((CAP,), N, jnp.int32).at[slot].set(flat_tok[order])
    slot_w = jnp.zeros((CAP,), jnp.float32).at[slot].set(flat_w[order])
    blk_expert = jnp.minimum(
        jnp.searchsorted(pend, jnp.arange(n_blocks, dtype=jnp.int32) * MOE_BLOCK, side='right'),
        N_EXPERTS - 1)
    h_pad = jnp.concatenate([hf, jnp.zeros((1, D), hf.dtype)], axis=0)

    def run(args):
        tok, e = args
        xb = h_pad[tok]
        return (jax.nn.silu(xb @ w1[e]) * (xb @ w3[e])) @ w2[e]

    out = lax.map(run, (slot_tok.reshape(n_blocks, MOE_BLOCK), blk_expert)).reshape(CAP, D)
    out = out * slot_w[:, None].astype(out.dtype)
    y = jax.ops.segment_sum(out, slot_tok, num_segments=N + 1)[:N]
    return y.reshape(B_, L, D).astype(h.dtype)


def setup_inputs(seed: int = 0) -> dict:
    key = jax.random.key(seed)
    ks = iter(jax.random.split(key, 40))
    D = D_MODEL
    nrm = lambda shape, scale: jax.random.normal(next(ks), shape, jnp.float32) * scale
    uni = lambda shape, lo, hi: jax.random.uniform(next(ks), shape, jnp.float32, lo, hi)
    return {
        "x": nrm((BATCH, SEQ, D), 1.0),
        "c": nrm((BATCH, D), 1.0),
        "ctx": nrm((BATCH, CTX_LEN, D), 1.0),
        "c_ctx": nrm((D,), 1.0),
        "ada_w": nrm((DEPTH, D, 6 * D), 0.3 * D ** -0.5),
        "ada_b": nrm((DEPTH, 6 * D), 0.02),
        "norm1_g": 1.0 + nrm((DEPTH, D), 0.02),
        "norm2_g": 1.0 + nrm((DEPTH, D), 0.02),
        "w_in": nrm((DEPTH, D, N_IN), D ** -0.5),
        "shift_mu": uni((DEPTH, RWKV_COLS), 0.0, 1.0),
        "rwkv_w0": uni((DEPTH, 2, RWKV_DIM), -6.0, -1.0),
        "rwkv_w2": nrm((DEPTH, 2, DECAY_LORA, RWKV_DIM), 0.5 * DECAY_LORA ** -0.5),
        "rwkv_a0": nrm((DEPTH, 2, RWKV_DIM), 0.1),
        "rwkv_a2": nrm((DEPTH, 2, AAA_LORA, RWKV_DIM), 0.5 * AAA_LORA ** -0.5),
        "rwkv_g2": nrm((DEPTH, GATE_LORA, RWKV_DIM), GATE_LORA ** -0.5),
        "rwkv_k_k": 0.85 + nrm((DEPTH, RWKV_DIM), 0.02),
        "rwkv_k_a": 1.0 + nrm((DEPTH, RWKV_DIM), 0.02),
        "rwkv_r_k": nrm((DEPTH, RWKV_HEADS, RWKV_HEAD_DIM), 0.1),
        "rwkv_lnx_g": 1.0 + nrm((DEPTH, RWKV_DIM), 0.02),
        "rwkv_lnx_b": nrm((DEPTH, RWKV_DIM), 0.02),
        "qn_g": 1.0 + nrm((DEPTH, DIFF_QK_DIM), 0.02),
        "kn_g": 1.0 + nrm((DEPTH, DIFF_QK_DIM), 0.02),
        "diff_lambda": nrm((DEPTH, 4, DIFF_QK_DIM), 0.1),
        "subln_g": 1.0 + nrm((DEPTH, DIFF_V_DIM), 0.02),
        "w_pa": nrm((DEPTH, RWKV_DIM, D), RWKV_DIM ** -0.5),
        "w_pb": nrm((DEPTH, DIFF_DIM, D), DIFF_DIM ** -0.5),
        "w_out": nrm((DEPTH, D, D), D ** -0.5),
        "router_g_w": nrm((DEPTH, D, N_GROUPS), D ** -0.5),
        "router_g_b": nrm((DEPTH, N_GROUPS), 0.01),
        "router_e_w": nrm((DEPTH, D, N_EXPERTS), D ** -0.5),
        "router_e_b": nrm((DEPTH, N_EXPERTS), 0.01),
        "exp_w1": nrm((DEPTH, N_EXPERTS, D, D_EXPERT), D ** -0.5),
        "exp_w3": nrm((DEPTH, N_EXPERTS, D, D_EXPERT), D ** -0.5),
        "exp_w2": nrm((DEPTH, N_EXPERTS, D_EXPERT, D), D_EXPERT ** -0.5),
    }


def reference(x, c, ctx, c_ctx, ada_w, ada_b, norm1_g, norm2_g, w_in, shift_mu,
              rwkv_w0, rwkv_w2, rwkv_a0, rwkv_a2, rwkv_g2, rwkv_k_k, rwkv_k_a, rwkv_r_k,
              rwkv_lnx_g, rwkv_lnx_b, qn_g, kn_g, diff_lambda, subln_g, w_pa, w_pb, w_out,
              router_g_w, router_g_b, router_e_w, router_e_b, exp_w1, exp_w3, exp_w2):
    B_, L, _ = x.shape
    ROWS = L // GRID_W
    rows = jnp.repeat(jnp.arange(ROWS, dtype=jnp.int32), GRID_W)
    cols = jnp.tile(jnp.arange(GRID_W, dtype=jnp.int32), ROWS)
    o_diff = RWKV_COLS
    o_gate = RWKV_COLS + DIFF_COLS
    cx = ctx
    for l in range(DEPTH):
        lam_init = 0.8 - 0.6 * math.exp(-0.3 * l)
        lv = diff_lambda[l].astype(jnp.float32)
        lam = jnp.exp(jnp.sum(lv[0] * lv[1])) - jnp.exp(jnp.sum(lv[2] * lv[3])) + lam_init
        mod_x = jax.nn.silu(c) @ ada_w[l] + ada_b[l]
        mod_c = jax.nn.silu(c_ctx) @ ada_w[l] + ada_b[l]
        sh1, sc1, gt1, sh2, sc2, gt2 = jnp.split(mod_x[:, None, :], 6, axis=-1)
        csh1, csc1, cgt1, csh2, csc2, cgt2 = jnp.split(mod_c, 6, axis=-1)
        hx = rmsnorm(x, norm1_g[l]) * (1.0 + sc1) + sh1
        hc = rmsnorm(cx, norm1_g[l]) * (1.0 + csc1) + csh1
        px = hx @ w_in[l]
        pc = hc @ w_in[l]
        rwkv_p = (rwkv_w0[l], rwkv_w2[l], rwkv_a0[l], rwkv_a2[l], rwkv_g2[l], rwkv_k_k[l],
                  rwkv_k_a[l], rwkv_r_k[l], rwkv_lnx_g[l], rwkv_lnx_b[l])
        s0 = jnp.zeros((B_, RWKV_HEADS, RWKV_HEAD_DIM, RWKV_HEAD_DIM), jnp.float32)
        yc_r, s_f, s_b = rwkv_branch(centred_shift(pc[..., :RWKV_COLS], shift_mu[l]), s0, s0, *rwkv_p)
        yx_r, _, _ = rwkv_branch(centred_shift(px[..., :RWKV_COLS], shift_mu[l]), s_f, s_b, *rwkv_p)
        qx, kx, vx = diff_qkv(px[..., o_diff:o_gate], qn_g[l], kn_g[l])
        qx = axial_rope(qx, rows, cols)
        kx = axial_rope(kx, rows, cols)
        qc, kc, vc = diff_qkv(pc[..., o_diff:o_gate], qn_g[l], kn_g[l])
        k_all = jnp.concatenate([kc, kx], axis=1)
        v_all = jnp.concatenate([vc, vx], axis=1)
        yx_d = diff_out(diff_attn_latent(qx, k_all, v_all, lam), subln_g[l], lam_init)
        mix_x = merge_branches(px[..., o_gate:], yx_r, yx_d, w_pa[l], w_pb[l], w_out[l])
        x_new = x + gt1 * mix_x
        moe_p = (router_g_w[l], router_g_b[l], router_e_w[l], router_e_b[l], exp_w1[l], exp_w3[l], exp_w2[l])
        h2 = rmsnorm(x_new, norm2_g[l]) * (1.0 + sc2) + sh2
        x_new = x_new + gt2 * hierarchical_moe(h2, *moe_p)
        if l < DEPTH - 1:
            yc_d = diff_out(diff_attn_block(qc, kc, vc, lam), subln_g[l], lam_init)
            mix_c = merge_branches(pc[..., o_gate:], yc_r, yc_d, w_pa[l], w_pb[l], w_out[l])
            cx = cx + cgt1 * mix_c
            hc2 = rmsnorm(cx, norm2_g[l]) * (1.0 + csc2) + csh2
            cx = cx + cgt2 * hierarchical_moe(hc2, *moe_p)
        x = x_new
    return x
```

```python
import numpy as np
from contextlib import ExitStack
import concourse.bass as bass
import concourse.mybir as mybir
from concourse.bass_utils import run_bass_kernel_spmd

F32 = mybir.dt.float32
BF16 = mybir.dt.bfloat16
I32 = mybir.dt.int32
ALU = mybir.AluOpType
AF = mybir.ActivationFunctionType
AX = mybir.AxisListType

P = 128
D = 2048
KC = 16
SEQ = 8192
CTX = 256
NTOK = SEQ + CTX
OWN = 4096
NEXP = 32
DEXP = 512

ENGS = ['pe', 'act', 'dve', 'pool', 'sp']
CWRAP = 30000
DK = 8
DWRAP = 3000


class Buf:
    __slots__ = ('w', 'r')

    def __init__(self):
        self.w = {}
        self.r = {}


def _evkey(ev):
    if ev[0] == 'c':
        return ('c', ev[1])
    return ('d', ev[1], ev[2] % DK)


class Sched:
    def __init__(self, nc, stack):
        self.nc = nc
        self.stack = stack
        self.ops = {e: [] for e in ENGS}
        self.cnt = {e: 0 for e in ENGS}
        self.dcnt = {e: 0 for e in ENGS}
        self.sems = {}
        self.waited = {e: {} for e in ENGS}
        self.last_dma = {}

    def sem(self, key):
        if key not in self.sems:
            name = "s_" + "_".join(str(k) for k in key)
            self.sems[key] = self.stack.enter_context(self.nc.semaphore(name))
        return self.sems[key]

    def _semval(self, ev):
        if ev[0] == 'c':
            return ('c', ev[1], ev[2] // CWRAP), ev[2] % CWRAP + 1
        n = ev[2]
        gen = n // DK
        return ('d', ev[1], n % DK, gen // DWRAP), 16 * (gen % DWRAP + 1)

    def op(self, eng, fn, reads=(), writes=(), dma=False):
        raw = {}
        oth = {}
        for b in reads:
            for k, v in b.w.items():
                if raw.get(k, -1) < v:
                    raw[k] = v
        for b in writes:
            for dd in (b.w, b.r):
                for k, v in dd.items():
                    if oth.get(k, -1) < v:
                        oth[k] = v
        if dma:
            n = self.dcnt[eng]
            self.dcnt[eng] += 1
            ev = ('d', eng, n)
            if n >= DK:
                k = ('d', eng, n % DK)
                if oth.get(k, -1) < n - DK:
                    oth[k] = n - DK
        else:
            idx = self.cnt[eng]
            self.cnt[eng] += 1
            ev = ('c', eng, idx)
        deps = {}
        for k, v in raw.items():
            if (not dma) and k == ('c', eng) and eng == 'pe':
                continue
            deps[k] = v
        for k, v in oth.items():
            if (not dma) and k == ('c', eng):
                continue
            if deps.get(k, -1) < v:
                deps[k] = v
        waits = []
        wd = self.waited[eng]
        for k, v in deps.items():
            e2 = ('c', k[1], v) if k[0] == 'c' else ('d', k[1], v)
            sk, sv = self._semval(e2)
            if wd.get(sk, 0) >= sv:
                continue
            wd[sk] = sv
            waits.append((self.sem(sk), sv))
        sk, sv = self._semval(ev)
        self.ops[eng].append((waits, fn, self.sem(sk), 16 if dma else 1))
        kk = _evkey(ev)
        for b in writes:
            b.w = {kk: ev[2]}
            b.r = {}
        for b in reads:
            if b.r.get(kk, -1) < ev[2]:
                b.r[kk] = ev[2]
        if dma:
            self.last_dma[kk] = ev[2]
        return ev

    def barrier(self):
        allb = Buf()
        for e in ENGS:
            if self.cnt[e] > 0:
                allb.w[('c', e)] = self.cnt[e] - 1
        for k, v in self.last_dma.items():
            allb.w[k] = v
        for e in ENGS:
            raw = dict(allb.w)
            waits = []
            wd = self.waited[e]
            for k, v in raw.items():
                if k == ('c', e):
                    continue
                e2 = ('c', k[1], v) if k[0] == 'c' else ('d', k[1], v)
                sk, sv = self._semval(e2)
                if wd.get(sk, 0) >= sv:
                    continue
                wd[sk] = sv
                waits.append((self.sem(sk), sv))
            if waits:
                idx = self.cnt[e]
                self.cnt[e] += 1
                sk, sv = self._semval(('c', e, idx))
                self.ops[e].append((waits, lambda en: en.nop(), self.sem(sk), 1))

    def emit(self):
        nc = self.nc
        with nc.Block() as blk:
            def replay(name):
                def f(en):
                    for waits, fn, sm, inc in self.ops[name]:
                        for (ws, wv) in waits:
                            en.wait_ge(ws, wv)
                        ins = fn(en)
                        ins.then_inc(sm, inc)
                return f
            blk.tensor(replay('pe'))
            blk.scalar(replay('act'))
            blk.vector(replay('dve'))
            blk.gpsimd(replay('pool'))
            blk.sync(replay('sp'))


class Arena:
    def __init__(self, nc, stack, name, nelem):
        self.t = stack.enter_context(nc.sbuf_tensor(name, [P, nelem], F32))
        self.n = nelem
        self.off = 0
        self.marks = []

    def alloc(self, *shape, dt=F32):
        n = int(np.prod(shape))
        n32 = n if dt == F32 else (n + 1) // 2
        assert self.off + n32 <= self.n, (self.off, n32, self.n)
        ap = self.t[:, self.off:self.off + n32]
        self.off += n32
        if dt != F32:
            ap = ap.bitcast(dt)
        if len(shape) == 2:
            ap = ap.rearrange("p (a b) -> p a b", a=shape[0])
        elif len(shape) == 3:
            ap = ap.rearrange("p (a b c) -> p a b c", a=shape[0], b=shape[1])
        return ap

    def mark(self):
        self.marks.append(self.off)

    def release(self):
        self.off = self.marks.pop()


class _B16:
    def __init__(self, ar):
        self.ar = ar

    def alloc(self, *shape):
        return self.ar.alloc(*shape, dt=BF16)

    def mark(self):
        pass

    def release(self):
        pass


T_R, T_K, T_V = 0, 8, 16
T_XW, T_XA, T_XG = 24, 26, 28
N_RW = 30
T_Q = 30
T_KK = 38
T_G = 46
N_FT = 78
PRW = NTOK + 4
PR_CTX = 1
PR_LAT = CTX + 3


def build(debug=None):
    nc = bass.Bass("TRN2", target_bir_lowering=False)
    stack = ExitStack()
    S = Sched(nc, stack)
    dbg_out = {}

    def din(name, shape, dt=F32):
        return nc.dram_tensor(name, list(shape), dt, kind="ExternalInput").ap()

    def dscr(name, shape, dt=F32):
        kind = "ExternalOutput" if (debug and name in debug) else "Internal"
        t = nc.dram_tensor(name, list(shape), dt, kind=kind).ap()
        return t

    x_loc = din("x_loc", [SEQ, D])
    ctx_loc = din("ctx_loc", [CTX, D])
    cT = din("cT", [P, KC * 2])
    adaT = din("adaT", [24, P, KC * 512])
    adab = din("adab", [1, 6 * D])
    n1g = din("n1g", [1, D])
    n2g = din("n2g", [1, D])
    winT = din("winT", [N_FT, P, KC * P])
    winV = din("winV", [8, P, KC * P])
    ident_in = din("ident", [P, P])
    bones_in = din("bones", [P, P])
    rotm_in = din("rotm", [P, P])
    cos_in = din("cosT", [P, SEQ])
    sin_in = din("sinT", [P, SEQ])
    qkg_in = din("qkg", [P, 2])
    dlam_in = din("dlam", [1, 256])
    subg_in = din("subg", [1, P])
    mk_in = din("mk", [P, 2])
    mu_in = din("mu", [P, N_RW])
    w2s_in = din("w2s", [2, P, 1024])
    a2s_in = din("a2s", [2, P, 1024])
    g2s_in = din("g2s", [P, 2 * 1024])
    chp_in = din("chp", [P, 8 * 8])
    lnx_in = din("lnx", [64, 16 * 2])
    maskA_in = din("maskA", [2, 64, 320])
    rst_in = din("rst", [P, 512])
    ones64_in = din("ones64", [64, 64])
    wpa_in = din("wpa", [1024, D])
    wpb_in = din("wpb", [1024, D])
    wout_in = din("wout", [D, D])
    wr_in = din("wr", [P, KC * 36])
    rbias_in = din("rbias", [1, 36])
    w1T_in = din("w1T", [NEXP, P, KC * DEXP])
    w3T_in = din("w3T", [NEXP, P, KC * DEXP])
    w2T_in = din("w2T", [NEXP, P, 4 * D])
    out = nc.dram_tensor("out", [OWN, D], F32, kind="ExternalOutput").ap()

    MODS = dscr("MODS", [12, D])
    PR = dscr("PR", [N_RW, P, PRW])
    QT = dscr("QT", [8, P, OWN])
    KT = dscr("KT", [8, P, NTOK])
    VV = dscr("VV", [NTOK, 1024])
    GT = dscr("GT", [32, P, OWN])
    YD = dscr("YD", [8, P, OWN], BF16)
    NCH = NTOK // 64
    KBd = dscr("KBd", [2, 1024, NCH * 128])
    KRd = dscr("KRd", [2, 1024, NCH * 128])
    VSd = dscr("VSd", [1024, NCH * 64])
    GCd = dscr("GCd", [2, 1024, NCH])
    BON = dscr("BON", [1024, OWN])
    GGd = dscr("GGd", [1024, OWN])
    YR = dscr("YR", [1024, OWN], BF16)
    MX = dscr("MX", [16, P, OWN], BF16)
    XN = dscr("XN", [OWN, D])
    H2T = dscr("H2T", [KC, P, OWN], BF16)
    GMT = dscr("GMT", [NEXP, OWN])
    W1B = dscr("W1B", [NEXP, P, KC * DEXP], BF16)
    W3B = dscr("W3B", [NEXP, P, KC * DEXP], BF16)
    W2B = dscr("W2B", [NEXP, P, 4 * D], BF16)

    AF32 = Arena(nc, stack, "arena", 49152)
    AB16 = _B16(AF32)
    ps = [stack.enter_context(nc.psum_tensor(f"ps{i}", [P, 512], F32)) for i in range(8)]
    psb = [ps[6][:, :].bitcast(BF16), ps[7][:, :].bitcast(BF16)]
    bps = [Buf() for _ in range(8)]
    bpsb = [bps[6], bps[7]]

    def mm(out_, lhsT, rhs, reads, writes, start=True, stop=True):
        S.op('pe', lambda e: e.matmul(out_, lhsT=lhsT, rhs=rhs, start=start, stop=stop), reads=reads, writes=writes)

    def tr(out_, in_, idn, reads, writes):
        S.op('pe', lambda e: e.transpose(out=out_, in_=in_, identity=idn), reads=reads, writes=writes)

    def tt(eng, out_, in0, in1, op, reads, writes):
        S.op(eng, lambda e: e.tensor_tensor(out=out_, in0=in0, in1=in1, op=op), reads=reads, writes=writes)

    def ts(eng, out_, in0, s1, s2, op0, op1, reads, writes):
        if s2 is None:
            S.op(eng, lambda e: e.tensor_scalar(out=out_, in0=in0, scalar1=s1, scalar2=None, op0=op0), reads=reads, writes=writes)
        else:
            S.op(eng, lambda e: e.tensor_scalar(out=out_, in0=in0, scalar1=s1, scalar2=s2, op0=op0, op1=op1), reads=reads, writes=writes)

    def stt(eng, out_, in0, sc, in1, op0, op1, reads, writes):
        S.op(eng, lambda e: e.scalar_tensor_tensor(out=out_, in0=in0, scalar=sc, in1=in1, op0=op0, op1=op1), reads=reads, writes=writes)

    def act(out_, in_, func, reads, writes, scale=1.0, bias=0.0, accum_out=None):
        if accum_out is None:
            S.op('act', lambda e: e.activation(out=out_, in_=in_, func=func, scale=scale, bias=bias), reads=reads, writes=writes)
        else:
            S.op('act', lambda e: e.activation(out=out_, in_=in_, func=func, scale=scale, bias=bias, accum_out=accum_out), reads=reads, writes=writes)

    def cp(eng, out_, in_, reads, writes):
        if eng == 'act':
            S.op(eng, lambda e: e.activation(out=out_, in_=in_, func=AF.Copy), reads=reads, writes=writes)
        else:
            S.op(eng, lambda e: e.tensor_copy(out=out_, in_=in_), reads=reads, writes=writes)

    def rcp(out_, in_, reads, writes):
        S.op('dve', lambda e: e.reciprocal(out=out_, in_=in_), reads=reads, writes=writes)

    def dma(out_, in_, reads=(), writes=None, slow=False):
        w = [Buf()] if writes is None else writes
        if slow:
            S.op('sp', lambda e: e.dma_start(out=out_, in_=in_, allow_slow_non_contiguous=True), reads=reads, writes=w, dma=True)
        else:
            S.op('sp', lambda e: e.dma_start(out=out_, in_=in_), reads=reads, writes=w, dma=True)

    ident = AF32.alloc(P)
    b_ident = Buf()
    identb = AB16.alloc(P)
    b_identb = Buf()
    S.op('sp', lambda e: e.dma_start(out=ident, in_=ident_in), writes=[b_ident], dma=True)
    S.op('dve', lambda e: e.tensor_copy(out=identb, in_=ident), reads=[b_ident], writes=[b_identb])

    AF32.mark(); AB16.mark()
    cs = AF32.alloc(KC, 2)
    b_cs = Buf()
    S.op('sp', lambda e: e.dma_start(out=cs, in_=cT.rearrange("p (a b) -> p a b", a=KC)), writes=[b_cs], dma=True)
    S.op('act', lambda e: e.activation(out=cs, in_=cs, func=AF.Silu), reads=[b_cs], writes=[b_cs])
    modrows = AF32.alloc(6 * D)
    b_mod = Buf()
    brow = AF32.alloc(6 * D)
    b_brow = Buf()
    for r in range(2):
        S.op('sp', (lambda r: lambda e: e.dma_start(out=brow[r:r + 1, :], in_=adab))(r), writes=[b_brow], dma=True)
    wA = [AF32.alloc(KC, 512) for _ in range(2)]
    b_wA = [Buf(), Buf()]
    for j in range(24):
        w = wA[j % 2]; bw = b_wA[j % 2]
        S.op('sp', (lambda w, j: lambda e: e.dma_start(out=w, in_=adaT[j].rearrange("p (a b) -> p a b", a=KC)))(w, j),
             writes=[bw], dma=True)
        pb = j % 2
        for kc in range(KC):
            S.op('pe', (lambda w, kc, pb: lambda e: e.matmul(ps[pb][0:2, :], lhsT=cs[:, kc, :], rhs=w[:, kc, :],
                                                              start=(kc == 0), stop=(kc == KC - 1)))(w, kc, pb),
                 reads=[b_cs, bw], writes=[bps[pb]])
        S.op('dve', (lambda j, pb: lambda e: e.tensor_tensor(out=modrows[0:2, j * 512:(j + 1) * 512], in0=ps[pb][0:2, :],
                                                             in1=brow[0:2, j * 512:(j + 1) * 512], op=ALU.add))(j, pb),
             reads=[bps[pb], b_brow], writes=[b_mod])
    for r in range(2):
        S.op('sp', (lambda r: lambda e: e.dma_start(out=brow[r:r + 1, 0:D], in_=n1g))(r), writes=[b_brow], dma=True)
        S.op('sp', (lambda r: lambda e: e.dma_start(out=brow[r:r + 1, D:2 * D], in_=n2g))(r), writes=[b_brow], dma=True)
    S.op('dve', lambda e: e.scalar_tensor_tensor(out=modrows[0:2, D:2 * D], in0=modrows[0:2, D:2 * D], scalar=1.0,
                                                 in1=brow[0:2, 0:D], op0=ALU.add, op1=ALU.mult),
         reads=[b_mod, b_brow], writes=[b_mod])
    S.op('dve', lambda e: e.scalar_tensor_tensor(out=modrows[0:2, 4 * D:5 * D], in0=modrows[0:2, 4 * D:5 * D], scalar=1.0,
                                                 in1=brow[0:2, D:2 * D], op0=ALU.add, op1=ALU.mult),
         reads=[b_mod, b_brow], writes=[b_mod])
    b_MODS = Buf()
    for (mi, col) in ((0, 1), (2, 0), (4, 4), (6, 3), (8, 2), (10, 5)):
        S.op('sp', (lambda mi, col: lambda e: e.dma_start(out=MODS[mi:mi + 2, :], in_=modrows[0:2, col * D:(col + 1) * D]))(mi, col),
             reads=[b_mod], writes=[b_MODS], dma=True)
    S.barrier()
    AF32.release(); AB16.release()

    if debug and debug.get('stop') == 'A':
        return finish(nc, S, stack)

    AF32.mark(); AB16.mark()
    Abc = [AF32.alloc(D) for _ in range(2)]
    Bbc = [AF32.alloc(D) for _ in range(2)]
    b_AB = Buf()
    for r in range(2):
        S.op('sp', (lambda r: lambda e: e.dma_start(out=Abc[r], in_=MODS[r:r + 1, :].partition_broadcast(P)))(r),
             reads=[b_MODS], writes=[b_AB], dma=True)
        S.op('sp', (lambda r: lambda e: e.dma_start(out=Bbc[r], in_=MODS[2 + r:3 + r, :].partition_broadcast(P)))(r),
             reads=[b_MODS], writes=[b_AB], dma=True)
    TS = 2048
    xt = [AF32.alloc(D) for _ in range(2)]; b_xt = [Buf(), Buf()]
    hxb = [AB16.alloc(D) for _ in range(2)]; b_hxb = [Buf(), Buf()]
    junk = AB16.alloc(D); b_junk = Buf()
    stat = [AF32.alloc(2) for _ in range(2)]; b_stat = [Buf(), Buf()]
    hT = AB16.alloc(KC, TS); b_hT = Buf()
    wf = [AF32.alloc(KC, P) for _ in range(2)]; b_wf = [Buf(), Buf()]
    wb = [AB16.alloc(KC, P) for _ in range(2)]; b_wb = [Buf(), Buf()]
    stg = [AF32.alloc(512) for _ in range(3)]; b_stg = [Buf() for _ in range(3)]
    zero = AF32.alloc(4); b_zero = Buf()
    S.op('dve', lambda e: e.memset(zero, 0.0), writes=[b_zero])
    b_PR = Buf(); b_QT = Buf(); b_KT = Buf(); b_VV = Buf(); b_GT = Buf()
    for j in range(N_RW):
        for c in (0, PR_CTX + CTX, PR_CTX + CTX + 1, PRW - 1):
            S.op('sp', (lambda j, c: lambda e: e.dma_start(out=PR[j, :, c:c + 1], in_=zero[:, 0:1], allow_slow_non_contiguous=True))(j, c),
                 reads=[b_zero], writes=[Buf()], dma=True)

    cnt = {'x': 0, 'w': 0, 'ps': 0, 'stg': 0, 'pb': 0}

    supers = [(ctx_loc, 0, CTX, 1, PR_CTX, 0, False)]
    for s4 in range(SEQ // TS):
        supers.append((x_loc, s4 * TS, TS, 0, PR_LAT + s4 * TS, CTX + s4 * TS, s4 * TS < OWN))
    if debug and debug.get('nsuper'):
        supers = supers[:debug['nsuper']]
    def do_super(src, row0, ntok, kind, prc0, kt0, own):
        for tt in range(ntok // P):
            i = cnt['x'] % 2; cnt['x'] += 1
            X = xt[i]; bX = b_xt[i]; H = hxb[i]; bH = b_hxb[i]; st = stat[i]; bst = b_stat[i]
            S.op('sp', (lambda X, r0: lambda e: e.dma_start(out=X, in_=src[r0:r0 + P, :]))(X, row0 + tt * P),
                 writes=[bX], dma=True)
            S.op('act', (lambda X, st: lambda e: e.activation(out=junk, in_=X, func=AF.Square, accum_out=st[:, 0:1]))(X, st),
                 reads=[bX], writes=[b_junk, bst])
            S.op('act', (lambda st: lambda e: e.activation(out=st[:, 1:2], in_=st[:, 0:1], func=AF.Sqrt, scale=1.0 / D, bias=1e-6))(st),
                 reads=[bst], writes=[bst])
            S.op('dve', (lambda st: lambda e: e.reciprocal(out=st[:, 1:2], in_=st[:, 1:2]))(st), reads=[bst], writes=[bst])
            S.op('dve', (lambda X, st: lambda e: e.scalar_tensor_tensor(out=X, in0=X, scalar=st[:, 1:2], in1=Abc[kind],
                                                                        op0=ALU.mult, op1=ALU.mult))(X, st),
                 reads=[bX, bst, b_AB], writes=[bX])
            S.op('dve', (lambda X, H: lambda e: e.tensor_tensor(out=H, in0=X, in1=Bbc[kind], op=ALU.add))(X, H),
                 reads=[bX, b_AB], writes=[bH])
            for half in range(2):
                pb = cnt['pb'] % 2; cnt['pb'] += 1
                for q in range(8):
                    kc = half * 8 + q
                    S.op('pe', (lambda H, kc, q, pb: lambda e: e.transpose(out=psb[pb][:, q * P:(q + 1) * P],
                                                                          in_=H[:, kc * P:(kc + 1) * P], identity=identb))(H, kc, q, pb),
                         reads=[bH, b_identb], writes=[bpsb[pb]])
                S.op('act' if half == 0 else 'dve',
                     (lambda half, pb, tt: lambda e: (e.activation(out=hT[:, half * 8:half * 8 + 8, tt * P:(tt + 1) * P],
                                                                   in_=psb[pb].rearrange("p (a b) -> p a b", a=8), func=AF.Copy)
                                                      if half == 0 else
                                                      e.tensor_copy(out=hT[:, half * 8:half * 8 + 8, tt * P:(tt + 1) * P],
                                                                    in_=psb[pb].rearrange("p (a b) -> p a b", a=8))))(half, pb, tt),
                     reads=[bpsb[pb]], writes=[b_hT])
        tiles = list(range(N_RW)) + list(range(T_KK, T_KK + 8))
        if own:
            tiles += list(range(T_Q, T_Q + 8)) + list(range(T_G, T_G + 32))
        if debug and debug.get('ntiles'):
            tiles = tiles[:debug['ntiles']]
        if debug and debug.get('vonly'):
            tiles = []
        nch = max(1, ntok // 512)
        cw = min(512, ntok)
        for j in tiles:
            i = cnt['w'] % 2; cnt['w'] += 1
            S.op('sp', (lambda i, j: lambda e: e.dma_start(out=wf[i], in_=winT[j].rearrange("p (a b) -> p a b", a=KC)))(i, j),
                 writes=[b_wf[i]], dma=True)
            S.op('pool', (lambda i: lambda e: e.tensor_copy(out=wb[i], in_=wf[i]))(i), reads=[b_wf[i]], writes=[b_wb[i]])
            for ch in range(nch):
                pi = cnt['ps'] % 4; cnt['ps'] += 1
                for kc in range(KC):
                    S.op('pe', (lambda i, kc, pi, ch: lambda e: e.matmul(ps[pi][:, 0:cw], lhsT=wb[i][:, kc, :],
                                                                          rhs=hT[:, kc, ch * 512:ch * 512 + cw],
                                                                          start=(kc == 0), stop=(kc == KC - 1)))(i, kc, pi, ch),
                         reads=[b_wb[i], b_hT], writes=[bps[pi]])
                si = cnt['stg'] % 3; cnt['stg'] += 1
                isg = j >= T_G
                S.op('act', (lambda pi, si, isg: lambda e: e.activation(out=stg[si][:, 0:cw], in_=ps[pi][:, 0:cw],
                                                                         func=(AF.Sigmoid if isg else AF.Copy)))(pi, si, isg),
                     reads=[bps[pi]], writes=[b_stg[si]])
                if j < N_RW:
                    dst = PR[j, :, prc0 + ch * 512:prc0 + ch * 512 + cw]; bd = b_PR
                elif j < T_KK:
                    dst = QT[j - T_Q, :, row0 + ch * 512:row0 + ch * 512 + cw]; bd = b_QT
                elif j < T_G:
                    dst = KT[j - T_KK, :, kt0 + ch * 512:kt0 + ch * 512 + cw]; bd = b_KT
                else:
                    dst = GT[j - T_G, :, row0 + ch * 512:row0 + ch * 512 + cw]; bd = b_GT
                S.op('sp', (lambda dst, si: lambda e: e.dma_start(out=dst, in_=stg[si][:, 0:cw]))(dst, si),
                     reads=[b_stg[si]], writes=[Buf()], dma=True)
        if not (debug and debug.get('ntiles')):
            for vj in range(8):
                i = cnt['w'] % 2; cnt['w'] += 1
                S.op('sp', (lambda i, vj: lambda e: e.dma_start(out=wf[i], in_=winV[vj].rearrange("p (a b) -> p a b", a=KC)))(i, vj),
                     writes=[b_wf[i]], dma=True)
                S.op('pool', (lambda i: lambda e: e.tensor_copy(out=wb[i], in_=wf[i]))(i), reads=[b_wf[i]], writes=[b_wb[i]])
                for t4 in range(0, ntok // P, 4):
                    n4 = min(4, ntok // P - t4)
                    pi = cnt['ps'] % 4; cnt['ps'] += 1
                    for q in range(n4):
                        for kc in range(KC):
                            S.op('pe', (lambda i, kc, pi, q, t4: lambda e: e.matmul(ps[pi][:, q * P:(q + 1) * P],
                                                                                    lhsT=hT[:, kc, (t4 + q) * P:(t4 + q + 1) * P],
                                                                                    rhs=wb[i][:, kc, :],
                                                                                    start=(kc == 0), stop=(kc == KC - 1)))(i, kc, pi, q, t4),
                                 reads=[b_wb[i], b_hT], writes=[bps[pi]])
                    si = cnt['stg'] % 3; cnt['stg'] += 1
                    S.op('act', (lambda pi, si, n4: lambda e: e.activation(out=stg[si][:, 0:n4 * P], in_=ps[pi][:, 0:n4 * P], func=AF.Copy))(pi, si, n4),
                         reads=[bps[pi]], writes=[b_stg[si]])
                    dst = VV[kt0 + t4 * P:kt0 + (t4 + n4) * P, vj * P:(vj + 1) * P].rearrange("(q p) c -> p q c", p=P)
                    S.op('sp', (lambda dst, si, n4: lambda e: e.dma_start(out=dst, in_=stg[si][:, 0:n4 * P].rearrange("p (q c) -> p q c", q=n4)))(dst, si, n4),
                         reads=[b_stg[si]], writes=[Buf()], dma=True)
    for sp_ in supers:
        do_super(*sp_)
    S.barrier()
    AF32.release(); AB16.release()
    if debug and debug.get('stop') == 'B':
        return finish(nc, S, stack)


    AF32.mark()
    bones = AF32.alloc(P); rotm = AF32.alloc(P); qkg = AF32.alloc(2); b_cF = Buf()
    S.op('sp', lambda e: e.dma_start(out=bones, in_=bones_in), writes=[b_cF], dma=True)
    S.op('sp', lambda e: e.dma_start(out=rotm, in_=rotm_in), writes=[b_cF], dma=True)
    S.op('sp', lambda e: e.dma_start(out=qkg, in_=qkg_in), writes=[b_cF], dma=True)
    dl = AF32.alloc(256); dl2 = AF32.alloc(2, 64); lam = AF32.alloc(4); subg = AF32.alloc(P); b_lam = Buf()
    S.op('sp', lambda e: e.dma_start(out=dl, in_=dlam_in.partition_broadcast(P)), writes=[b_lam], dma=True)
    S.op('sp', lambda e: e.dma_start(out=subg, in_=subg_in.partition_broadcast(P)), writes=[b_lam], dma=True)
    dlv = dl.rearrange("p (a b c) -> p a b c", a=2, b=2)
    S.op('dve', lambda e: e.tensor_tensor(out=dl2, in0=dlv[:, :, 0, :], in1=dlv[:, :, 1, :], op=ALU.mult), reads=[b_lam], writes=[b_lam])
    S.op('dve', lambda e: e.tensor_reduce(out=lam[:, 0:2], in_=dl2, axis=AX.X, op=ALU.add), reads=[b_lam], writes=[b_lam])
    S.op('act', lambda e: e.activation(out=lam[:, 0:2], in_=lam[:, 0:2], func=AF.Exp), reads=[b_lam], writes=[b_lam])
    S.op('dve', lambda e: e.scalar_tensor_tensor(out=lam[:, 2:3], in0=lam[:, 1:2], scalar=-0.2, in1=lam[:, 0:1], op0=ALU.add, op1=ALU.subtract),
         reads=[b_lam], writes=[b_lam])
    S.op('dve', lambda e: e.tensor_scalar(out=subg, in0=subg, scalar1=0.8, scalar2=None, op0=ALU.mult), reads=[b_lam], writes=[b_lam])
    Kb = AF32.alloc(2, NTOK, dt=BF16); b_Kb = Buf()
    kfull = AF32.alloc(512, dt=BF16); b_kfull = Buf()
    mk = AF32.alloc(2);
    S.op('sp', lambda e: e.dma_start(out=mk, in_=mk_in), writes=[b_cF], dma=True)
    Vb = AF32.alloc(NTOK // P, 132, dt=BF16); b_Vb = Buf()
    S.op('pool', lambda e: e.memset(Vb[:, :, 128:129], 1.0), writes=[b_Vb])
    tin = [AF32.alloc(512) for _ in range(2)]; b_tin = [Buf(), Buf()]
    tcs = [AF32.alloc(2, 512) for _ in range(2)]; b_tcs = [Buf(), Buf()]
    tsq = AF32.alloc(512); b_tsq = Buf()
    trs = AF32.alloc(512); b_trs = Buf()
    tkn = AF32.alloc(512); b_tkn = Buf()
    Qb = [AF32.alloc(512, dt=BF16) for _ in range(2)]; b_Qb = [Buf(), Buf()]
    PT = [AF32.alloc(512, dt=BF16) for _ in range(3)]; b_PT = [Buf() for _ in range(3)]
    vin = [AF32.alloc(P) for _ in range(2)]; b_vin = [Buf(), Buf()]
    ot = [AF32.alloc(2, 132) for _ in range(2)]; b_ot = [Buf(), Buf()]
    osm = [AF32.alloc(8) for _ in range(2)]
    yb = [AF32.alloc(P, dt=BF16) for _ in range(2)]; b_yb = [Buf(), Buf()]
    ydt = [AF32.alloc(512, dt=BF16) for _ in range(2)]; b_ydt = [Buf(), Buf()]
    cF = {'t': 0, 'q': 0, 'pt': 0, 'v': 0, 'o': 0, 'sc': 0, 'yd': 0}

    def qk_prep(srcap, n, dst, b_dst, gcol, tab0):
        i = cF['t'] % 2; cF['t'] += 1
        T = tin[i]; bT = b_tin[i]; CS = tcs[i]; bCS = b_tcs[i]
        S.op('sp', lambda e: e.dma_start(out=T[:, 0:n], in_=srcap), writes=[bT], dma=True)
        if tab0 is not None:
            S.op('sp', lambda e: e.dma_start(out=CS[:, 0, 0:n], in_=cos_in[:, tab0:tab0 + n]), writes=[bCS], dma=True)
            S.op('sp', lambda e: e.dma_start(out=CS[:, 1, 0:n], in_=sin_in[:, tab0:tab0 + n]), writes=[bCS], dma=True)
        S.op('act', lambda e: e.activation(out=tsq[:, 0:n], in_=T[:, 0:n], func=AF.Square), reads=[bT], writes=[b_tsq])
        S.op('pe', lambda e: e.matmul(ps[4][:, 0:n], lhsT=bones, rhs=tsq[:, 0:n], start=True, stop=True), reads=[b_tsq, b_cF], writes=[bps[4]])
        S.op('act', lambda e: e.activation(out=trs[:, 0:n], in_=ps[4][:, 0:n], func=AF.Sqrt, scale=1.0 / 64, bias=1e-6), reads=[bps[4]], writes=[b_trs])
        S.op('dve', lambda e: e.reciprocal(out=trs[:, 0:n], in_=trs[:, 0:n]), reads=[b_trs], writes=[b_trs])
        if tab0 is None:
            S.op('dve', lambda e: e.scalar_tensor_tensor(out=dst, in0=T[:, 0:n], scalar=qkg[:, gcol:gcol + 1], in1=trs[:, 0:n], op0=ALU.mult, op1=ALU.mult),
                 reads=[bT, b_trs, b_cF], writes=[b_dst])
            return
        S.op('dve', lambda e: e.scalar_tensor_tensor(out=tkn[:, 0:n], in0=T[:, 0:n], scalar=qkg[:, gcol:gcol + 1], in1=trs[:, 0:n], op0=ALU.mult, op1=ALU.mult),
             reads=[bT, b_trs, b_cF], writes=[b_tkn])
        S.op('pe', lambda e: e.matmul(ps[5][:, 0:n], lhsT=rotm, rhs=tkn[:, 0:n], start=True, stop=True), reads=[b_tkn, b_cF], writes=[bps[5]])
        S.op('dve', lambda e: e.tensor_tensor(out=CS[:, 1, 0:n], in0=ps[5][:, 0:n], in1=CS[:, 1, 0:n], op=ALU.mult), reads=[bps[5], bCS], writes=[bCS])
        S.op('pool', lambda e: e.tensor_tensor(out=CS[:, 0, 0:n], in0=tkn[:, 0:n], in1=CS[:, 0, 0:n], op=ALU.mult), reads=[b_tkn, bCS], writes=[bCS])
        S.op('dve', lambda e: e.tensor_tensor(out=dst, in0=CS[:, 0, 0:n], in1=CS[:, 1, 0:n], op=ALU.add), reads=[bCS], writes=[b_dst])

    def attn_head(h):
        def ksplit(c0, n):
            for m in range(2):
                S.op('dve' if m == 0 else 'pool', (lambda m: lambda e: e.tensor_scalar(out=Kb[:, m, c0:c0 + n], in0=kfull[:, 0:n], scalar1=mk[:, m:m + 1],
                                                                                 scalar2=None, op0=ALU.mult))(m),
                     reads=[b_kfull, b_cF], writes=[b_Kb])
        qk_prep(KT[h, :, 0:CTX], CTX, kfull[:, 0:CTX], b_kfull, 1, None)
        ksplit(0, CTX)
        for c in range(SEQ // 512):
            qk_prep(KT[h, :, CTX + c * 512:CTX + (c + 1) * 512], 512, kfull, b_kfull, 1, c * 512)
            ksplit(CTX + c * 512, 512)
        for t in range(NTOK // P):
            i = cF['v'] % 2; cF['v'] += 1
            S.op('sp', (lambda i, t: lambda e: e.dma_start(out=vin[i], in_=VV[t * P:(t + 1) * P, h * P:(h + 1) * P]))(i, t), writes=[b_vin[i]], dma=True)
            S.op('pool', (lambda i, t: lambda e: e.tensor_copy(out=Vb[:, t, 0:P], in_=vin[i]))(i, t), reads=[b_vin[i]], writes=[b_Vb])
        QC = 256
        nq = OWN // QC
        NKT = (debug or {}).get('nkt') or NTOK // P
        if debug and debug.get('nq'):
            nq = debug['nq']
        for qc in range(nq):
            qi = cF['q'] % 2; cF['q'] += 1
            qk_prep(QT[h, :, qc * QC:(qc + 1) * QC], QC, Qb[qi][:, 0:QC], b_Qb[qi], 0, qc * QC)
            for kt in range(NKT):
                for m in range(2):
                    sc = cF['sc'] % 2; cF['sc'] += 1
                    S.op('pe', (lambda m, kt, qi, sc: lambda e: e.matmul(ps[4 + sc][:, 0:QC], lhsT=Kb[:, m, kt * P:(kt + 1) * P],
                                                                        rhs=Qb[qi][:, 0:QC], start=True, stop=True))(m, kt, qi, sc),
                         reads=[b_Kb, b_Qb[qi]], writes=[bps[4 + sc]])
                    pi = cF['pt'] % 3; cF['pt'] += 1
                    S.op('act', (lambda sc, pi: lambda e: e.activation(out=PT[pi][:, 0:QC], in_=ps[4 + sc][:, 0:QC], func=AF.Exp, scale=0.125))(sc, pi),
                         reads=[bps[4 + sc]], writes=[b_PT[pi]])
                    for qs in range(2):
                        S.op('pe', (lambda m, kt, pi, qs: lambda e: e.matmul(ps[qs * 2 + m][:, 0:129], lhsT=PT[pi][:, qs * P:(qs + 1) * P],
                                                                            rhs=Vb[:, kt, 0:129], start=(kt == 0), stop=(kt == NKT - 1)))(m, kt, pi, qs),
                             reads=[b_PT[pi], b_Vb], writes=[bps[qs * 2 + m]])
            yi = cF['yd'] % 2; cF['yd'] += 1
            for qs in range(2):
                oi = cF['o'] % 2; cF['o'] += 1
                O = ot[oi]; bO = b_ot[oi]; sm = osm[oi]
                for m in range(2):
                    S.op('dve', (lambda O, qs, m: lambda e: e.tensor_copy(out=O[:, m, 0:129], in_=ps[qs * 2 + m][:, 0:129]))(O, qs, m),
                         reads=[bps[qs * 2 + m]], writes=[bO])
                S.op('dve', (lambda O, sm: lambda e: e.reciprocal(out=sm[:, 0:2], in_=O[:, :, 128]))(O, sm), reads=[bO], writes=[bO])
                S.op('dve', (lambda sm: lambda e: e.tensor_tensor(out=sm[:, 1:2], in0=sm[:, 1:2], in1=lam[:, 2:3], op=ALU.mult))(sm), reads=[bO, b_lam], writes=[bO])
                S.op('dve', (lambda O, sm: lambda e: e.tensor_scalar(out=O[:, 0, 0:P], in0=O[:, 0, 0:P], scalar1=sm[:, 0:1], scalar2=None, op0=ALU.mult))(O, sm),
                     reads=[bO], writes=[bO])
                S.op('dve', (lambda O, sm: lambda e: e.scalar_tensor_tensor(out=O[:, 0, 0:P], in0=O[:, 1, 0:P], scalar=sm[:, 1:2], in1=O[:, 0, 0:P],
                                                                            op0=ALU.mult, op1=ALU.add))(O, sm), reads=[bO], writes=[bO])
                S.op('act', (lambda O, sm: lambda e: e.activation(out=O[:, 1, 0:P], in_=O[:, 0, 0:P], func=AF.Square, accum_out=sm[:, 2:3]))(O, sm),
                     reads=[bO], writes=[bO])
                S.op('act', (lambda sm: lambda e: e.activation(out=sm[:, 3:4], in_=sm[:, 2:3], func=AF.Sqrt, scale=1.0 / P, bias=1e-5))(sm), reads=[bO], writes=[bO])
                S.op('dve', (lambda sm: lambda e: e.reciprocal(out=sm[:, 3:4], in_=sm[:, 3:4]))(sm), reads=[bO], writes=[bO])
                S.op('dve', (lambda O, sm, oi: lambda e: e.scalar_tensor_tensor(out=yb[oi], in0=O[:, 0, 0:P], scalar=sm[:, 3:4], in1=subg,
                                                                                op0=ALU.mult, op1=ALU.mult))(O, sm, oi),
                     reads=[bO, b_lam], writes=[b_yb[oi]])
                pb = cnt['pb'] % 2; cnt['pb'] += 1
                S.op('pe', (lambda oi, pb: lambda e: e.transpose(out=psb[pb][:, 0:P], in_=yb[oi], identity=identb))(oi, pb),
                     reads=[b_yb[oi], b_identb], writes=[bpsb[pb]])
                S.op('act', (lambda pb, yi, qs: lambda e: e.activation(out=ydt[yi][:, qs * P:(qs + 1) * P], in_=psb[pb][:, 0:P], func=AF.Copy))(pb, yi, qs),
                     reads=[bpsb[pb]], writes=[b_ydt[yi]])
            S.op('sp', (lambda yi, qc: lambda e: e.dma_start(out=YD[h, :, qc * QC:(qc + 1) * QC], in_=ydt[yi][:, 0:QC]))(yi, qc),
                 reads=[b_ydt[yi]], writes=[Buf()], dma=True)

    nheads = 8
    if debug and debug.get('nheads'):
        nheads = debug['nheads']
    if debug and debug.get('skipF'):
        nheads = 0
    for h in range(nheads):
        attn_head(h)
    S.barrier()
    AF32.release()
    if debug and debug.get('stop') == 'F':
        return finish(nc, S, stack)

    C0 = 0.6065306597126334
    if not (debug and debug.get('skipC')):
        AF32.mark()
        mu = AF32.alloc(N_RW); chp = AF32.alloc(8, 8); bonesC = AF32.alloc(P); rstm = AF32.alloc(512)
        w2s = AF32.alloc(2, 1024); a2s = AF32.alloc(2, 1024); g2s = AF32.alloc(2, 1024)
        omka = AF32.alloc(8)
        b_cC = Buf()
        dma(mu, mu_in, writes=[b_cC]); dma(chp, chp_in.rearrange("p (a b) -> p a b", a=8), writes=[b_cC])
        dma(bonesC, bones_in, writes=[b_cC]); dma(rstm, rst_in, writes=[b_cC])
        dma(w2s, w2s_in.rearrange("d p c -> p d c"), writes=[b_cC]); dma(a2s, a2s_in.rearrange("d p c -> p d c"), writes=[b_cC])
        dma(g2s, g2s_in.rearrange("p (a b) -> p a b", a=2), writes=[b_cC])
        ts('dve', omka, chp[:, :, 5], -1.0, 1.0, ALU.mult, ALU.add, [b_cC], [b_cC])
        NB = 514
        raw = [AF32.alloc(NB) for _ in range(3)]; b_raw = [Buf() for _ in range(3)]
        tmpS = [AF32.alloc(512) for _ in range(2)]; b_tmpS = [Buf(), Buf()]
        lx = [AF32.alloc(512) for _ in range(6)]; b_lx = [Buf() for _ in range(6)]
        rr = AF32.alloc(512); kk_ = AF32.alloc(512); vv_ = AF32.alloc(512); kn_ = AF32.alloc(512)
        b_rr = Buf(); b_k = Buf(); b_v = Buf(); b_kn = Buf()
        sg = [AF32.alloc(512) for _ in range(2)]; aa = [AF32.alloc(512) for _ in range(2)]
        b_sg = [Buf(), Buf()]; b_aa = [Buf(), Buf()]
        kd = [AF32.alloc(512) for _ in range(2)]; b_kd = [Buf(), Buf()]
        bb = AF32.alloc(512); b_bb = Buf()
        csg = AF32.alloc(512); b_csg = Buf()
        cpv = AF32.alloc(512); b_cpv = Buf()
        gm = AF32.alloc(512); gp = AF32.alloc(512); gq = AF32.alloc(512); b_gm = Buf(); b_gp = Buf(); b_gq = Buf()
        gcs = AF32.alloc(8); b_gcs = Buf()
        o4 = [AF32.alloc(512) for _ in range(4)]; b_o4 = [Buf() for _ in range(4)]
        t1 = AF32.alloc(512); b_t1 = Buf()
        gst = AF32.alloc(512); b_gst = Buf()
        cC = {'raw': 0, 'tmp': 0, 'o': 0}

        def load_shift(j, pc0, n, dst, b_dst, post=None):
            i = cC['raw'] % 3; cC['raw'] += 1
            R = raw[i]; bR = b_raw[i]
            ti = cC['tmp'] % 2; cC['tmp'] += 1
            T = tmpS[ti]; bT = b_tmpS[ti]
            dma(R[:, 0:n + 2], PR[j, :, pc0 - 1:pc0 + n + 1], writes=[bR])
            tt('pool', T[:, 0:n], R[:, 0:n], R[:, 2:n + 2], ALU.add, [bR], [bT])
            stt('dve', T[:, 0:n], T[:, 0:n], 0.5, R[:, 1:n + 1], ALU.mult, ALU.subtract, [bT, bR], [bT])
            if post is None:
                stt('dve', dst[:, 0:n], T[:, 0:n], mu[:, j:j + 1], R[:, 1:n + 1], ALU.mult, ALU.add, [bT, bR, b_cC], [b_dst])
            else:
                stt('dve', T[:, 0:n], T[:, 0:n], mu[:, j:j + 1], R[:, 1:n + 1], ALU.mult, ALU.add, [bT, bR, b_cC], [bT])
                act(dst[:, 0:n], T[:, 0:n], post, [bT], [b_dst])

        def phaseC_tile(pc0, n, c0, own0):
            nch = n // 64
            load_shift(T_XW + 0, pc0, n, lx[0], b_lx[0], AF.Tanh)
            load_shift(T_XW + 1, pc0, n, lx[1], b_lx[1], AF.Tanh)
            load_shift(T_XA + 0, pc0, n, lx[2], b_lx[2], AF.Copy)
            load_shift(T_XA + 1, pc0, n, lx[3], b_lx[3], AF.Copy)
            load_shift(T_XG + 0, pc0, n, lx[4], b_lx[4], AF.Sigmoid)
            load_shift(T_XG + 1, pc0, n, lx[5], b_lx[5], AF.Sigmoid)
            for ct in range(8):
                cs_ = slice(ct * P, (ct + 1) * P)
                load_shift(T_R + ct, pc0, n, rr, b_rr)
                load_shift(T_K + ct, pc0, n, kk_, b_k)
                load_shift(T_V + ct, pc0, n, vv_, b_v)
                dma(VSd[cs_, c0 * 64:c0 * 64 + n], vv_[:, 0:n], reads=[b_v])
                for d in range(2):
                    mm(ps[d][:, 0:n], w2s[0:96, d, cs_], lx[d][0:96, 0:n], [b_cC, b_lx[d]], [bps[d]])
                    act(sg[d][:, 0:n], ps[d][:, 0:n], AF.Sigmoid, [bps[d], b_cC], [b_sg[d]], bias=chp[:, ct, d:d + 1])
                    mm(ps[2 + d][:, 0:n], a2s[0:96, d, cs_], lx[2 + d][0:96, 0:n], [b_cC, b_lx[2 + d]], [bps[2 + d]])
                    act(aa[d][:, 0:n], ps[2 + d][:, 0:n], AF.Sigmoid, [bps[2 + d], b_cC], [b_aa[d]], bias=chp[:, ct, 2 + d:3 + d])
                if own0 is not None:
                    mm(ps[4][:, 0:n], g2s[:, 0, cs_], lx[4][:, 0:n], [b_cC, b_lx[4]], [bps[4]], start=True, stop=False)
                    mm(ps[4][:, 0:n], g2s[:, 1, cs_], lx[5][:, 0:n], [b_cC, b_lx[5]], [bps[4]], start=False, stop=True)
                    act(gst[:, 0:n], ps[4][:, 0:n], AF.Copy, [bps[4]], [b_gst])
                    dma(GGd[cs_, own0:own0 + n], gst[:, 0:n], reads=[b_gst])
                ts('dve', kn_[:, 0:n], kk_[:, 0:n], chp[:, ct, 4:5], None, ALU.mult, None, [b_k, b_cC], [b_kn])
                act(t1[:, 0:n], kn_[:, 0:n], AF.Square, [b_kn], [b_t1])
                mm(ps[5][:, 0:n], bonesC, t1[:, 0:n], [b_cC, b_t1], [bps[5]])
                act(t1[:, 0:n], ps[5][:, 0:n], AF.Sqrt, [bps[5]], [b_t1], bias=1e-12)
                rcp(t1[:, 0:n], t1[:, 0:n], [b_t1], [b_t1])
                tt('dve', kn_[:, 0:n], kn_[:, 0:n], t1[:, 0:n], ALU.mult, [b_kn, b_t1], [b_kn])
                for d in range(2):
                    ts('pool', kd[d][:, 0:n], aa[d][:, 0:n], chp[:, ct, 5:6], omka[:, ct:ct + 1], ALU.mult, ALU.add, [b_aa[d], b_cC], [b_kd[d]])
                    tt('pool', kd[d][:, 0:n], kd[d][:, 0:n], kk_[:, 0:n], ALU.mult, [b_kd[d], b_k], [b_kd[d]])
                    tt('pool', bb[:, 0:n], kn_[:, 0:n], aa[d][:, 0:n], ALU.mult, [b_kn, b_aa[d]], [b_bb])
                    S.op('dve', (lambda d: lambda e: e.tensor_tensor_scan(out=csg[:, 0:n], data0=rstm[:, 0:n], data1=sg[d][:, 0:n], initial=0.0,
                                                                           op0=ALU.mult, op1=ALU.add))(d),
                         reads=[b_cC, b_sg[d]], writes=[b_csg])
                    c3 = csg[:, 0:n].rearrange("p (a b) -> p a b", b=64)
                    if d == 0:
                        tt('dve', cpv[:, 0:n], csg[:, 0:n], sg[d][:, 0:n], ALU.subtract, [b_csg, b_sg[d]], [b_cpv])
                        cum = csg; b_cum = b_csg
                        act(gcs[:, 0:nch], c3[:, :, 63], AF.Exp, [b_csg], [b_gcs], scale=-C0)
                    else:
                        act(gcs[:, 0:nch], c3[:, :, 63], AF.Exp, [b_csg], [b_gcs], scale=-C0)
                        stt('dve', cpv[:, 0:n].rearrange("p (a b) -> p a b", b=64), c3, -1.0, c3[:, :, 63:64].to_broadcast([P, nch, 64]),
                            ALU.mult, ALU.add, [b_csg], [b_cpv])
                        tt('dve', csg[:, 0:n], cpv[:, 0:n], sg[d][:, 0:n], ALU.add, [b_cpv, b_sg[d]], [b_csg])
                        cum = csg; b_cum = b_csg
                    dma(GCd[d, cs_, c0:c0 + nch], gcs[:, 0:nch], reads=[b_gcs])
                    act(gm[:, 0:n], cum[:, 0:n], AF.Exp, [b_cum], [b_gm], scale=C0)
                    act(gq[:, 0:n], cum[:, 0:n], AF.Exp, [b_cum], [b_gq], scale=-C0)
                    act(gp[:, 0:n], cpv[:, 0:n], AF.Exp, [b_cpv], [b_gp], scale=-C0)
                    oi = [cC['o'] % 4, (cC['o'] + 1) % 4, (cC['o'] + 2) % 4, (cC['o'] + 3) % 4]; cC['o'] += 4
                    tt('dve', o4[oi[0]][:, 0:n], kd[d][:, 0:n], gm[:, 0:n], ALU.mult, [b_kd[d], b_gm], [b_o4[oi[0]]])
                    tt('pool', o4[oi[1]][:, 0:n], bb[:, 0:n], gm[:, 0:n], ALU.mult, [b_bb, b_gm], [b_o4[oi[1]]])
                    tt('dve', o4[oi[2]][:, 0:n], kn_[:, 0:n], gp[:, 0:n], ALU.mult, [b_kn, b_gp], [b_o4[oi[2]]])
                    tt('pool', o4[oi[3]][:, 0:n], rr[:, 0:n], gq[:, 0:n], ALU.mult, [b_rr, b_gq], [b_o4[oi[3]]])
                    KBv = KBd[d, cs_, c0 * 128:(c0 + nch) * 128].rearrange("p (a b) -> p a b", b=128)
                    KRv = KRd[d, cs_, c0 * 128:(c0 + nch) * 128].rearrange("p (a b) -> p a b", b=128)
                    dma(KBv[:, :, 0:64], o4[oi[0]][:, 0:n].rearrange("p (a b) -> p a b", b=64), reads=[b_o4[oi[0]]])
                    dma(KBv[:, :, 64:128], o4[oi[1]][:, 0:n].rearrange("p (a b) -> p a b", b=64), reads=[b_o4[oi[1]]])
                    dma(KRv[:, :, 0:64], o4[oi[2]][:, 0:n].rearrange("p (a b) -> p a b", b=64), reads=[b_o4[oi[2]]])
                    dma(KRv[:, :, 64:128], o4[oi[3]][:, 0:n].rearrange("p (a b) -> p a b", b=64), reads=[b_o4[oi[3]]])
                if own0 is not None:
                    tt('dve', t1[:, 0:n], kd[0][:, 0:n], kd[1][:, 0:n], ALU.add, [b_kd[0], b_kd[1]], [b_t1])
                    stt('dve', t1[:, 0:n], rr[:, 0:n], chp[:, ct, 6:7], t1[:, 0:n], ALU.mult, ALU.mult, [b_rr, b_cC, b_t1], [b_t1])
                    mm(ps[5][:, 0:n], bonesC, t1[:, 0:n], [b_cC, b_t1], [bps[5]])
                    tt('dve', gst[:, 0:n], ps[5][:, 0:n], vv_[:, 0:n], ALU.mult, [bps[5], b_v], [b_gst])
                    dma(BON[cs_, own0:own0 + n], gst[:, 0:n], reads=[b_gst])

        phaseC_tile(PR_CTX, CTX, 0, None)
        nlt = (debug or {}).get('nlt') or SEQ // 512
        for lt in range(nlt):
            phaseC_tile(PR_LAT + lt * 512, 512, 4 + lt * 8, lt * 512 if lt * 512 < OWN else None)
        S.barrier()
        AF32.release()
    if debug and debug.get('stop') == 'C':
        return finish(nc, S, stack)

    AF32.mark()
    id64 = ident[0:64, 0:64]
    id4 = AF32.alloc(4, 64); maskA = AF32.alloc(2, 320); ones64 = AF32.alloc(64); lnx = AF32.alloc(16, 2); b_cD = Buf()
    for u in range(4):
        cp('dve', id4[0:64, u, :], id64, [b_ident], [b_cD])
    dma(maskA[0:64], maskA_in.rearrange("d p c -> p d c"), writes=[b_cD])
    dma(ones64[0:64], ones64_in, writes=[b_cD]); dma(lnx[0:64], lnx_in.rearrange("p (a b) -> p a b", a=16), writes=[b_cD])
    S0T = AF32.alloc(4, 64); b_S0 = Buf()
    gct = AF32.alloc(4, NCH); b_gct = Buf()
    YT = AF32.alloc(4, OWN); b_YT = Buf()
    GL = 4
    kbL = [AF32.alloc(4, GL * 128) for _ in range(2)]; b_kbL = [Buf(), Buf()]
    krL = [AF32.alloc(4, GL * 128) for _ in range(2)]; b_krL = [Buf(), Buf()]
    vvL = [AF32.alloc(4, GL * 64) for _ in range(2)]; b_vvL = [Buf(), Buf()]
    atb = [AF32.alloc(4, 320) for _ in range(2)]; b_at = [Buf(), Buf()]
    Xb = [AF32.alloc(4, 64) for _ in range(2)]; b_X = [Buf(), Buf()]
    pq = [AF32.alloc(4, 128) for _ in range(2)]; b_pq = [Buf(), Buf()]
    kbt = [AF32.alloc(4, 128) for _ in range(2)]; b_kbt = [Buf(), Buf()]
    vtt = [AF32.alloc(4, 64) for _ in range(2)]; b_vtt = [Buf(), Buf()]
    wm = AF32.alloc(4, 64); b_wm = Buf()
    nsa = AF32.alloc(4, 64); b_nsa = Buf()
    ey = [AF32.alloc(512) for _ in range(2)]; b_ey = [Buf(), Buf()]
    esq = AF32.alloc(512); b_esq = Buf()
    ers = AF32.alloc(512); b_ers = Buf()
    ebo = [AF32.alloc(512) for _ in range(2)]; b_ebo = [Buf(), Buf()]
    egg = [AF32.alloc(512) for _ in range(2)]; b_egg = [Buf(), Buf()]
    eout = [AF32.alloc(512, dt=BF16) for _ in range(2)]; b_eout = [Buf(), Buf()]
    NSTEP = (debug or {}).get('nstep') or 132
    NFWD = min(68, NSTEP)

    def scan_ct(ct):
        def rows(u):
            hh = u % 2
            return slice((2 * ct + hh) * 64, (2 * ct + hh + 1) * 64)
        S.op('dve', lambda e: e.memset(S0T[0:64], 0.0), writes=[b_S0])
        for u in range(4):
            dma(gct[0:64, u, :], GCd[u // 2, rows(u), :], writes=[b_gct])

        def chunk_of(u, i):
            if u < 2:
                return i
            return 3 - i if i < 4 else 135 - i

        def active(u, i):
            return (u >= 2) or (i < NFWD)

        def load_group(g):
            li = g % 2
            for u in range(4):
                if not active(u, 4 * g):
                    continue
                if u < 2:
                    cst = 4 * g
                else:
                    cst = 0 if g == 0 else 132 - 4 * g
                d = u // 2
                dma(kbL[li][0:64, u, :], KBd[d, rows(u), cst * 128:(cst + GL) * 128], writes=[b_kbL[li]])
                dma(krL[li][0:64, u, :], KRd[d, rows(u), cst * 128:(cst + GL) * 128], writes=[b_krL[li]])
                dma(vvL[li][0:64, u, :], VSd[rows(u), cst * 64:(cst + GL) * 64], writes=[b_vvL[li]])

        def views(u, i):
            g = i // 4; li = g % 2
            if u < 2:
                j = i - 4 * g
            else:
                cst = 0 if g == 0 else 132 - 4 * g
                j = chunk_of(u, i) - cst
            KB = kbL[li][0:64, u, j * 128:(j + 1) * 128]
            KR = krL[li][0:64, u, j * 128:(j + 1) * 128]
            V = vvL[li][0:64, u, j * 64:(j + 1) * 64]
            return KB, KR, V, [b_kbL[li], b_krL[li], b_vvL[li]]

        def prep(i):
            bi = i % 2
            AT = atb[bi]; bAT = b_at[bi]; X = Xb[bi]; bX = b_X[bi]
            us = [u for u in range(4) if active(u, i)]
            u0, u1 = us[0], us[-1] + 1
            for u in us:
                KB, KR, V, bl = views(u, i)
                pa = ps[u % 2]; bpa = bps[u % 2]
                mm(pa[0:64, 0:128], KB[:, 0:64], KR[:, 0:128], bl, [bpa])
                mm(pa[0:64, 128:256], KB[:, 64:128], KR[:, 0:128], bl, [bpa])
                mm(pa[0:64, 256:320], KR[:, 0:64], KB[:, 64:128], bl, [bpa])
                tt('dve', AT[0:64, u, :], pa[0:64, 0:320], maskA[0:64, u // 2, :], ALU.mult, [bpa, b_cD], [bAT])
                tr(ps[4][0:64, u * 128:u * 128 + 64], KB[:, 0:64], id64, bl + [b_ident], [bps[4]])
                tr(ps[4][0:64, u * 128 + 64:u * 128 + 128], KB[:, 64:128], id64, bl + [b_ident], [bps[4]])
                tr(ps[5][0:64, u * 64:(u + 1) * 64], V, id64, bl + [b_ident], [bps[5]])
            cp('act', kbt[bi][0:64, u0:u1, :], ps[4][0:64, u0 * 128:u1 * 128].rearrange("p (a b) -> p a b", b=128), [bps[4]], [b_kbt[bi]])
            cp('act', vtt[bi][0:64, u0:u1, :], ps[5][0:64, u0 * 64:u1 * 64].rearrange("p (a b) -> p a b", b=64), [bps[5]], [b_vtt[bi]])
            tt('dve', X[0:64, u0:u1, :], id4[0:64, u0:u1, :], AT[0:64, u0:u1, 128:192], ALU.subtract, [b_cD, bAT], [bX])
            for it in range(5):
                pj = it % 2
                for u in us:
                    if it == 0:
                        Pm = AT[0:64, u, 128:192]; Qm = AT[0:64, u, 256:320]; bsrc = bAT
                    else:
                        Pm = pq[1 - pj][0:64, u, 0:64]; Qm = pq[1 - pj][0:64, u, 64:128]; bsrc = b_pq[1 - pj]
                    mm(ps[2][0:64, u * 128:u * 128 + 64], Qm, Pm, [bsrc], [bps[2]])
                    mm(ps[2][0:64, u * 128 + 64:u * 128 + 128], Pm, Qm, [bsrc], [bps[2]])
                cp('act', pq[pj][0:64, u0:u1, :], ps[2][0:64, u0 * 128:u1 * 128].rearrange("p (a b) -> p a b", b=128), [bps[2]], [b_pq[pj]])
                for u in us:
                    mm(ps[3][0:64, u * 64:(u + 1) * 64], pq[pj][0:64, u, 64:128], X[0:64, u, :], [b_pq[pj], bX], [bps[3]])
                tt('dve', X[0:64, u0:u1, :], X[0:64, u0:u1, :], ps[3][0:64, u0 * 64:u1 * 64].rearrange("p (a b) -> p a b", b=64), ALU.add,
                   [bX, bps[3]], [bX])

        def chain(i):
            bi = i % 2
            AT = atb[bi]; bAT = b_at[bi]; X = Xb[bi]; bX = b_X[bi]
            KT_ = kbt[bi]; bKT = b_kbt[bi]; VT = vtt[bi]; bVT = b_vtt[bi]
            us = [u for u in range(4) if active(u, i)]
            u0, u1 = us[0], us[-1] + 1
            for u in us:
                KB, KR, V, bl = views(u, i)
                mm(ps[6][0:64, u * 64:(u + 1) * 64], KR[:, 0:64], S0T[0:64, u, :], bl + [b_S0], [bps[6]], start=True, stop=False)
                mm(ps[6][0:64, u * 64:(u + 1) * 64], AT[0:64, u, 0:64], VT[0:64, u, :], [bAT, bVT], [bps[6]], start=False, stop=True)
            cp('act', wm[0:64, u0:u1, :], ps[6][0:64, u0 * 64:u1 * 64].rearrange("p (a b) -> p a b", b=64), [bps[6]], [b_wm])
            for u in us:
                mm(ps[7][0:64, u * 64:(u + 1) * 64], X[0:64, u, :], wm[0:64, u, :], [bX, b_wm], [bps[7]])
            ts('dve', nsa[0:64, u0:u1, :], ps[7][0:64, u0 * 64:u1 * 64].rearrange("p (a b) -> p a b", b=64), -1.0, None, ALU.mult, None,
               [bps[7]], [b_nsa])
            need_y = [u for u in us if 4 <= chunk_of(u, i) < 4 + OWN // 64]
            for u in need_y:
                KB, KR, V, bl = views(u, i)
                mm(ps[2][0:64, u * 64:(u + 1) * 64], S0T[0:64, u, :], KR[:, 64:128], bl + [b_S0], [bps[2]], start=True, stop=False)
                mm(ps[2][0:64, u * 64:(u + 1) * 64], VT[0:64, u, :], AT[0:64, u, 64:128], [bVT, bAT], [bps[2]], start=False, stop=False)
                mm(ps[2][0:64, u * 64:(u + 1) * 64], nsa[0:64, u, :], AT[0:64, u, 192:256], [b_nsa, bAT], [bps[2]], start=False, stop=True)
            for dd in range(2):
                uu = [u for u in need_y if u // 2 == dd]
                if not uu:
                    continue
                t0 = (chunk_of(uu[0], i) - 4) * 64
                cp('act', YT[0:64, uu[0]:uu[-1] + 1, t0:t0 + 64],
                   ps[2][0:64, uu[0] * 64:(uu[-1] + 1) * 64].rearrange("p (a b) -> p a b", b=64), [bps[2]], [b_YT])
            for u in us:
                mm(ps[3][0:64, u * 64:(u + 1) * 64], KT_[0:64, u, 0:64], VT[0:64, u, :], [bKT, bVT], [bps[3]], start=True, stop=False)
                mm(ps[3][0:64, u * 64:(u + 1) * 64], KT_[0:64, u, 64:128], nsa[0:64, u, :], [bKT, b_nsa], [bps[3]], start=False, stop=False)
                mm(ps[3][0:64, u * 64:(u + 1) * 64], id64, S0T[0:64, u, :], [b_ident, b_S0], [bps[3]], start=False, stop=True)
            for dd in range(2):
                uu = [u for u in us if u // 2 == dd]
                if not uu:
                    continue
                c = chunk_of(uu[0], i)
                a, b2 = uu[0], uu[-1] + 1
                tt('dve', S0T[0:64, a:b2, :], ps[3][0:64, a * 64:b2 * 64].rearrange("p (a b) -> p a b", b=64),
                   gct[0:64, a:b2, c:c + 1].to_broadcast([64, b2 - a, 64]), ALU.mult, [bps[3], b_gct], [b_S0])

        load_group(0)
        prep(0)
        for i in range(NSTEP):
            if i + 1 < NSTEP:
                if (i + 1) % 4 == 0:
                    load_group((i + 1) // 4)
                prep(i + 1)
            chain(i)
        for hh in range(2):
            head = 2 * ct + hh
            hr = slice(head * 64, (head + 1) * 64)
            for c8 in range(OWN // 512):
                cs2 = slice(c8 * 512, (c8 + 1) * 512)
                i2 = c8 % 2
                Y = ey[i2]; bY = b_ey[i2]
                dma(ebo[i2][0:64], BON[hr, cs2], writes=[b_ebo[i2]])
                dma(egg[i2][0:64], GGd[hr, cs2], writes=[b_egg[i2]])
                tt('dve', Y[0:64], YT[0:64, hh, cs2], YT[0:64, 2 + hh, cs2], ALU.add, [b_YT], [bY])
                mm(ps[0][0:64, :], ones64[0:64], Y[0:64], [b_cD, bY], [bps[0]])
                tt('dve', Y[0:64], Y[0:64], ps[0][0:64, :], ALU.subtract, [bY, bps[0]], [bY])
                act(esq[0:64], Y[0:64], AF.Square, [bY], [b_esq])
                mm(ps[1][0:64, :], ones64[0:64], esq[0:64], [b_cD, b_esq], [bps[1]])
                act(ers[0:64], ps[1][0:64, :], AF.Sqrt, [bps[1]], [b_ers], bias=64e-5)
                rcp(ers[0:64], ers[0:64], [b_ers], [b_ers])
                tt('dve', Y[0:64], Y[0:64], ers[0:64], ALU.mult, [bY, b_ers], [bY])
                ts('dve', Y[0:64], Y[0:64], lnx[0:64, head, 0:1], lnx[0:64, head, 1:2], ALU.mult, ALU.add, [bY, b_cD], [bY])
                tt('pool', Y[0:64], Y[0:64], ebo[i2][0:64], ALU.add, [bY, b_ebo[i2]], [bY])
                tt('dve', eout[i2][0:64], Y[0:64], egg[i2][0:64], ALU.mult, [bY, b_egg[i2]], [b_eout[i2]])
                dma(YR[hr, cs2], eout[i2][0:64], reads=[b_eout[i2]])

    nct = (debug or {}).get('nct') or 8
    for ct in range(nct):
        scan_ct(ct)
    S.barrier()
    AF32.release()
    if debug and debug.get('stop') == 'E':
        return finish(nc, S, stack)

    AF32.mark()
    wpab = AF32.alloc(8, D, dt=BF16); wpbb = AF32.alloc(8, D, dt=BF16); b_wp = Buf()
    wst = [AF32.alloc(D) for _ in range(2)]; b_wst = [Buf(), Buf()]
    k_ = 0
    for (src_w, dstw) in ((wpa_in, wpab), (wpb_in, wpbb)):
        for kc in range(8):
            i = k_ % 2; k_ += 1
            dma(wst[i], src_w[kc * P:(kc + 1) * P, :], writes=[b_wst[i]])
            cp('pool' if kc % 2 else 'dve', dstw[:, kc, :], wst[i], [b_wst[i]], [b_wp])
    yrT = [AF32.alloc(8, 512, dt=BF16) for _ in range(2)]; b_yrT = [Buf(), Buf()]
    ydT = [AF32.alloc(8, 512, dt=BF16) for _ in range(2)]; b_ydT = [Buf(), Buf()]
    g0t = [AF32.alloc(512) for _ in range(2)]; g1t = [AF32.alloc(512) for _ in range(2)]
    b_g0t = [Buf(), Buf()]; b_g1t = [Buf(), Buf()]
    ta = [AF32.alloc(512) for _ in range(2)]; tb_ = [AF32.alloc(512) for _ in range(2)]
    b_ta = [Buf(), Buf()]; b_tb = [Buf(), Buf()]
    mxo = [AF32.alloc(512, dt=BF16) for _ in range(2)]; b_mxo = [Buf(), Buf()]
    YRv = YR.rearrange("(k p) t -> p k t", p=P)
    YDv = YD.rearrange("h p t -> p h t")
    NCK = (debug or {}).get('nck') or OWN // 512
    cg = 0
    for ck in range(NCK):
        cs3 = slice(ck * 512, (ck + 1) * 512)
        ci = ck % 2
        dma(yrT[ci], YRv[:, :, cs3], writes=[b_yrT[ci]])
        dma(ydT[ci], YDv[:, :, cs3], writes=[b_ydT[ci]])
        for ft in range(16):
            i = cg % 2; cg += 1
            dma(g0t[i], GT[ft, :, cs3], writes=[b_g0t[i]])
            dma(g1t[i], GT[16 + ft, :, cs3], writes=[b_g1t[i]])
            pa_ = ps[i]; pb_ = ps[2 + i]
            for kc in range(8):
                mm(pa_[:, :], wpab[:, kc, ft * P:(ft + 1) * P], yrT[ci][:, kc, :], [b_wp, b_yrT[ci]], [bps[i]], start=(kc == 0), stop=(kc == 7))
            for kc in range(8):
                mm(pb_[:, :], wpbb[:, kc, ft * P:(ft + 1) * P], ydT[ci][:, kc, :], [b_wp, b_ydT[ci]], [bps[2 + i]], start=(kc == 0), stop=(kc == 7))
            tt('dve', ta[i], pa_[:, :], g0t[i], ALU.mult, [bps[i], b_g0t[i]], [b_ta[i]])
            tt('dve', tb_[i], pb_[:, :], g1t[i], ALU.mult, [bps[2 + i], b_g1t[i]], [b_tb[i]])
            tt('pool', mxo[i], ta[i], tb_[i], ALU.add, [b_ta[i], b_tb[i]], [b_mxo[i]])
            dma(MX[ft, :, cs3], mxo[i], reads=[b_mxo[i]])
    S.barrier()
    AF32.release()

    AF32.mark()
    woutb = AF32.alloc(16, D, dt=BF16); b_wo = Buf()
    wst = [AF32.alloc(D) for _ in range(2)]; b_wst = [Buf(), Buf()]
    for ft in range(16):
        i = ft % 2
        dma(wst[i], wout_in[ft * P:(ft + 1) * P, :], writes=[b_wst[i]])
        cp('pool' if ft % 2 else 'dve', woutb[:, ft, :], wst[i], [b_wst[i]], [b_wo])
    gt1b = AF32.alloc(D); A2b = AF32.alloc(D); B2b = AF32.alloc(D); wr = AF32.alloc(KC, 36); rbias = AF32.alloc(36); b_cG = Buf()
    dma(gt1b, MODS[8:9, :].partition_broadcast(P), reads=[b_MODS], writes=[b_cG])
    dma(A2b, MODS[4:5, :].partition_broadcast(P), reads=[b_MODS], writes=[b_cG])
    dma(B2b, MODS[6:7, :].partition_broadcast(P), reads=[b_MODS], writes=[b_cG])
    dma(wr, wr_in.rearrange("p (a b) -> p a b", a=KC), writes=[b_cG])
    dma(rbias, rbias_in.partition_broadcast(P), writes=[b_cG])
    mxT = [AF32.alloc(16, P, dt=BF16) for _ in range(2)]; b_mxT = [Buf(), Buf()]
    xg_ = [AF32.alloc(D) for _ in range(2)]; b_xg = [Buf(), Buf()]
    xn_ = [AF32.alloc(D) for _ in range(2)]; b_xn = [Buf(), Buf()]
    h2 = AF32.alloc(D); b_h2 = Buf()
    junk2 = AF32.alloc(D, dt=BF16); b_junk2 = Buf()
    st2 = [AF32.alloc(4) for _ in range(2)]; b_st2 = [Buf(), Buf()]
    h2T32 = AF32.alloc(KC, P); b_h2T32 = Buf()
    h2Tb = [AF32.alloc(KC, P, dt=BF16) for _ in range(2)]; b_h2Tb = [Buf(), Buf()]
    rt = [AF32.alloc(160) for _ in range(2)]; b_rt = [Buf(), Buf()]
    gmo = [AF32.alloc(P) for _ in range(2)]; b_gmo = [Buf(), Buf()]
    MXv = MX.rearrange("f p t -> p f t")
    H2Tv = H2T.rearrange("k p t -> p k t")
    NTT = (debug or {}).get('ntt') or OWN // P
    for t_ in range(NTT):
        i = t_ % 2
        ts_ = slice(t_ * P, (t_ + 1) * P)
        dma(mxT[i], MXv[:, :, ts_], writes=[b_mxT[i]])
        dma(xg_[i], x_loc[ts_, :], writes=[b_xg[i]])
        XNt = xn_[i]; bXN = b_xn[i]
        for nt in range(4):
            ns = slice(nt * 512, (nt + 1) * 512)
            for ft in range(16):
                mm(ps[nt][:, :], mxT[i][:, ft, :], woutb[:, ft, ns], [b_mxT[i], b_wo], [bps[nt]], start=(ft == 0), stop=(ft == 15))
            tt('dve', XNt[:, ns], ps[nt][:, :], gt1b[:, ns], ALU.mult, [bps[nt], b_cG], [bXN])
        tt('pool', XNt, XNt, xg_[i], ALU.add, [bXN, b_xg[i]], [bXN])
        dma(XN[ts_, :], XNt, reads=[bXN])
        sq2 = st2[i]; bsq = b_st2[i]
        act(junk2, XNt, AF.Square, [bXN], [b_junk2, bsq], accum_out=sq2[:, 0:1])
        act(sq2[:, 1:2], sq2[:, 0:1], AF.Sqrt, [bsq], [bsq], scale=1.0 / D, bias=1e-6)
        rcp(sq2[:, 1:2], sq2[:, 1:2], [bsq], [bsq])
        stt('dve', h2, XNt, sq2[:, 1:2], A2b, ALU.mult, ALU.mult, [bXN, bsq, b_cG], [b_h2])
        tt('pool', h2, h2, B2b, ALU.add, [b_h2, b_cG], [b_h2])
        for q4 in range(4):
            pb_i = 4 + q4 % 2
            for q in range(4):
                kc = q4 * 4 + q
                tr(ps[pb_i][:, q * P:(q + 1) * P], h2[:, kc * P:(kc + 1) * P], ident, [b_h2, b_ident], [bps[pb_i]])
            cp('act' if q4 % 2 == 0 else 'dve', h2T32[:, q4 * 4:q4 * 4 + 4, :], ps[pb_i][:, :].rearrange("p (a b) -> p a b", a=4), [bps[pb_i]], [b_h2T32])
        cp('pool', h2Tb[i], h2T32, [b_h2T32], [b_h2Tb[i]])
        dma(H2Tv[:, :, ts_], h2Tb[i], reads=[b_h2Tb[i]])
        for kc in range(KC):
            mm(ps[6][:, 0:36], h2T32[:, kc, :], wr[:, kc, :], [b_h2T32, b_cG], [bps[6]], start=(kc == 0), stop=(kc == KC - 1))
        R = rt[i]; bR = b_rt[i]
        L = R[:, 0:36]
        tt('dve', L, ps[6][:, 0:36], rbias, ALU.add, [bps[6], b_cG], [bR])
        sc_ = R[:, 136:160]
        S.op('dve', (lambda R, sc_: lambda e: e.tensor_reduce(out=sc_[:, 0:1], in_=R[:, 0:4], axis=AX.X, op=ALU.max))(R, sc_), reads=[bR], writes=[bR])
        ts('dve', R[:, 36:40], R[:, 0:4], sc_[:, 0:1], None, ALU.is_equal, None, [bR], [bR])
        ts('dve', sc_[:, 1:2], sc_[:, 0:1], -1.0, None, ALU.mult, None, [bR], [bR])
        act(R[:, 104:108], R[:, 0:4], AF.Exp, [bR], [bR], bias=sc_[:, 1:2], accum_out=sc_[:, 2:3])
        rcp(sc_[:, 3:4], sc_[:, 2:3], [bR], [bR])
        ts('dve', R[:, 36:40], R[:, 36:40], 1e30, -1e30, ALU.mult, ALU.add, [bR], [bR])
        tt('dve', R[:, 40:72].rearrange("p (a b) -> p a b", a=4), R[:, 4:36].rearrange("p (a b) -> p a b", a=4),
           R[:, 36:40].rearrange("p (a b) -> p a b", b=1).to_broadcast([P, 4, 8]), ALU.add, [bR], [bR])
        S.op('dve', (lambda R, sc_: lambda e: e.tensor_reduce(out=sc_[:, 4:5], in_=R[:, 40:72], axis=AX.X, op=ALU.max))(R, sc_), reads=[bR], writes=[bR])
        ts('dve', R[:, 72:104], R[:, 40:72], sc_[:, 4:5], None, ALU.is_equal, None, [bR], [bR])
        stt('dve', R[:, 104:136], R[:, 72:104], -1e30, R[:, 40:72], ALU.mult, ALU.add, [bR], [bR])
        S.op('dve', (lambda R, sc_: lambda e: e.tensor_reduce(out=sc_[:, 5:6], in_=R[:, 104:136], axis=AX.X, op=ALU.max))(R, sc_), reads=[bR], writes=[bR])
        ts('dve', R[:, 104:136], R[:, 104:136], sc_[:, 5:6], None, ALU.is_equal, None, [bR], [bR])
        tt('dve', sc_[:, 6:7], sc_[:, 4:5], sc_[:, 5:6], ALU.subtract, [bR], [bR])
        act(sc_[:, 7:8], sc_[:, 6:7], AF.Sigmoid, [bR], [bR])
        tt('dve', sc_[:, 8:9], sc_[:, 7:8], sc_[:, 3:4], ALU.mult, [bR], [bR])
        tt('dve', sc_[:, 9:10], sc_[:, 3:4], sc_[:, 8:9], ALU.subtract, [bR], [bR])
        ts('dve', R[:, 72:104], R[:, 72:104], sc_[:, 8:9], None, ALU.mult, None, [bR], [bR])
        stt('dve', R[:, 40:72], R[:, 104:136], sc_[:, 9:10], R[:, 72:104], ALU.mult, ALU.add, [bR], [bR])
        tr(ps[7][0:32, 0:P], R[:, 40:72], ident, [bR, b_ident], [bps[7]])
        cp('act', gmo[i][0:32, :], ps[7][0:32, 0:P], [bps[7]], [b_gmo[i]])
        dma(GMT[:, ts_], gmo[i][0:32, :], reads=[b_gmo[i]])
    S.barrier()
    AF32.release()
    if debug and debug.get('stop') == 'G':
        return finish(nc, S, stack)

    AF32.mark()
    wfs = [AF32.alloc(KC * DEXP) for _ in range(2)]; b_wfs = [Buf(), Buf()]
    wbs = [AF32.alloc(KC * DEXP, dt=BF16) for _ in range(2)]; b_wbs = [Buf(), Buf()]
    NE = (debug or {}).get('nexp') or NEXP
    k_ = 0
    for e_ in range(NE):
        for (srcw, dstw) in ((w1T_in, W1B), (w3T_in, W3B), (w2T_in, W2B)):
            i = k_ % 2
            dma(wfs[i], srcw[e_], writes=[b_wfs[i]])
            half = KC * DEXP // 2
            cp('dve', wbs[i][:, 0:half], wfs[i][:, 0:half], [b_wfs[i]], [b_wbs[i]])
            cp('pool' if k_ % 2 else 'act', wbs[i][:, half:], wfs[i][:, half:], [b_wfs[i]], [b_wbs[i]])
            dma(dstw[e_], wbs[i], reads=[b_wbs[i]])
            k_ += 1
    S.barrier()
    AF32.release()

    AF32.mark()
    ST = 1024
    h2s = AF32.alloc(KC, ST, dt=BF16); b_h2s = Buf()
    yacc = AF32.alloc(ST // P, D); b_yacc = Buf()
    w1b = AF32.alloc(KC, DEXP, dt=BF16); w3b = AF32.alloc(KC, DEXP, dt=BF16); w2b = AF32.alloc(4, D, dt=BF16)
    b_w1b = Buf(); b_w3b = Buf(); b_w2b = Buf()
    uT = AF32.alloc(4, ST, dt=BF16); b_uT = Buf()
    gb = [AF32.alloc(ST) for _ in range(2)]; b_gb = [Buf(), Buf()]
    s1 = [AF32.alloc(512) for _ in range(2)]; b_s1 = [Buf(), Buf()]
    t3 = [AF32.alloc(512) for _ in range(2)]; b_t3 = [Buf(), Buf()]
    gt2b = AF32.alloc(D); b_gt2 = Buf()
    xno = AF32.alloc(D); b_xno = Buf()
    dma(gt2b, MODS[10:11, :].partition_broadcast(P), reads=[b_MODS], writes=[b_gt2])
    b_out = Buf()
    NST = (debug or {}).get('nst') or OWN // ST
    kk2 = 0
    for st_ in range(NST):
        tsl = slice(st_ * ST, (st_ + 1) * ST)
        dma(h2s, H2Tv[:, :, tsl], writes=[b_h2s])
        for e_ in range(NE):
            gi = e_ % 2
            dma(gb[gi], GMT[e_:e_ + 1, tsl].partition_broadcast(P), writes=[b_gb[gi]])
            dma(w1b, W1B[e_].rearrange("p (a b) -> p a b", a=KC), writes=[b_w1b])
            dma(w3b, W3B[e_].rearrange("p (a b) -> p a b", a=KC), writes=[b_w3b])
            dma(w2b, W2B[e_].rearrange("p (a b) -> p a b", a=4), writes=[b_w2b])
            for dt_ in range(4):
                for tch in range(ST // 512):
                    i = kk2 % 2; kk2 += 1
                    cs4 = slice(tch * 512, (tch + 1) * 512)
                    for kc in range(KC):
                        mm(ps[i][:, :], w1b[:, kc, dt_ * P:(dt_ + 1) * P], h2s[:, kc, cs4], [b_w1b, b_h2s], [bps[i]], start=(kc == 0), stop=(kc == KC - 1))
                    for kc in range(KC):
                        mm(ps[2 + i][:, :], w3b[:, kc, dt_ * P:(dt_ + 1) * P], h2s[:, kc, cs4], [b_w3b, b_h2s], [bps[2 + i]], start=(kc == 0), stop=(kc == KC - 1))
                    act(s1[i], ps[i][:, :], AF.Silu, [bps[i]], [b_s1[i]])
                    tt('dve', t3[i], ps[2 + i][:, :], gb[gi][:, cs4], ALU.mult, [bps[2 + i], b_gb[gi]], [b_t3[i]])
                    tt('pool', uT[:, dt_, cs4], s1[i], t3[i], ALU.mult, [b_s1[i], b_t3[i]], [b_uT])
            for tt_ in range(ST // P):
                for nt in range(4):
                    pi = 4 + (tt_ * 4 + nt) % 4
                    ns = slice(nt * 512, (nt + 1) * 512)
                    for dt_ in range(4):
                        mm(ps[pi][:, :], uT[:, dt_, tt_ * P:(tt_ + 1) * P], w2b[:, dt_, ns], [b_uT, b_w2b], [bps[pi]], start=(dt_ == 0), stop=(dt_ == 3))
                    if e_ == 0:
                        cp('dve', yacc[:, tt_, ns], ps[pi][:, :], [bps[pi]], [b_yacc])
                    else:
                        tt('dve', yacc[:, tt_, ns], yacc[:, tt_, ns], ps[pi][:, :], ALU.add, [b_yacc, bps[pi]], [b_yacc])
        for tt_ in range(ST // P):
            r0 = st_ * ST + tt_ * P
            dma(xno, XN[r0:r0 + P, :], writes=[b_xno])
            tt('pool', yacc[:, tt_, :], yacc[:, tt_, :], gt2b, ALU.mult, [b_yacc, b_gt2], [b_yacc])
            tt('dve', xno, xno, yacc[:, tt_, :], ALU.add, [b_xno, b_yacc], [b_xno])
            dma(out[r0:r0 + P, :], xno, reads=[b_xno])
    AF32.release()

    return finish(nc, S, stack)


def finish(nc, S, stack):
    S.barrier()
    S.emit()
    return nc, stack


def win_layout(w_in, h):
    C = 1024
    cols = []
    cols.append(w_in[:, 0:3 * C])
    o = 3 * C
    xw = [w_in[:, o:o + 96], w_in[:, o + 96:o + 192]]; o += 192
    xa = [w_in[:, o:o + 96], w_in[:, o + 96:o + 192]]; o += 192
    xg = w_in[:, o:o + 256]; o += 256
    z32 = np.zeros((D, 32), np.float32)
    order = (0, 1) if h == 0 else (1, 0)
    for d in order:
        cols += [xw[d], z32]
    for d in order:
        cols += [xa[d], z32]
    cols.append(xg)
    q = w_in[:, o:o + 1024]; o += 1024
    k = w_in[:, o:o + 1024]; o += 1024
    v = w_in[:, o:o + 1024]; o += 1024
    g = w_in[:, o:o + 4096]
    cols += [q, k, g]
    W = np.concatenate(cols, axis=1)
    assert W.shape[1] == N_FT * P, W.shape
    winT = W.reshape(KC, P, N_FT, P).transpose(2, 1, 0, 3).reshape(N_FT, P, KC * P)
    winV = v.reshape(KC, P, 8, P).transpose(2, 1, 0, 3).reshape(8, P, KC * P)
    return np.ascontiguousarray(winT), np.ascontiguousarray(winV)


def make_inputs(inp, core):
    b, h = core // 2, core % 2
    x = inp['x'][b]; ctx = inp['ctx'][b]
    if h == 1:
        x = x[::-1]; ctx = ctx[::-1]
    m = {}
    m['x_loc'] = np.ascontiguousarray(x)
    m['ctx_loc'] = np.ascontiguousarray(ctx)
    cc = np.stack([inp['c'][b], inp['c_ctx']], axis=-1)
    m['cT'] = np.ascontiguousarray(cc.reshape(KC, P, 2).transpose(1, 0, 2).reshape(P, KC * 2))
    return m


def shared_inputs(inp, h, reuse=None):
    m = {}
    aw = inp['ada_w'][0]
    if reuse is None:
        m['adaT'] = np.ascontiguousarray(aw.reshape(KC, P, 24, 512).transpose(2, 1, 0, 3).reshape(24, P, KC * 512))
    else:
        m['adaT'] = reuse['adaT']
    m['adab'] = np.ascontiguousarray(inp['ada_b'][0][None, :])
    m['n1g'] = np.ascontiguousarray(inp['norm1_g'])
    m['n2g'] = np.ascontiguousarray(inp['norm2_g'])
    m['winT'], m['winV'] = win_layout(inp['w_in'][0], h)
    m['ident'] = np.eye(P, dtype=np.float32)
    bo = np.zeros((P, P), np.float32); bo[:64, :64] = 1; bo[64:, 64:] = 1
    m['bones'] = bo
    rm = np.zeros((P, P), np.float32)
    for mp in range(P):
        if (mp % 32) < 16:
            rm[mp + 16, mp] = -1.0
        else:
            rm[mp - 16, mp] = 1.0
    m['rotm'] = rm
    t = np.arange(SEQ)
    if h == 1:
        t = t[::-1]
    rows = (t // 64).astype(np.float32); colsp = (t % 64).astype(np.float32)
    half = 32
    inv_freq = (10000.0 ** (-np.arange(0, half, 2, dtype=np.float32) / half)).astype(np.float32)
    cosT = np.zeros((P, SEQ), np.float32); sinT = np.zeros((P, SEQ), np.float32)
    for p in range(P):
        d = p % 64
        pos = rows if d < 32 else colsp
        ang = (pos * inv_freq[d % 16]).astype(np.float32)
        cosT[p] = np.cos(ang); sinT[p] = np.sin(ang)
    m['cosT'] = cosT; m['sinT'] = sinT
    m['qkg'] = np.ascontiguousarray(np.stack([np.tile(inp['qn_g'][0], 2), np.tile(inp['kn_g'][0], 2)], axis=1))
    m['dlam'] = np.ascontiguousarray(inp['diff_lambda'][0].reshape(1, 256))
    m['subg'] = np.ascontiguousarray(inp['subln_g'])
    mkk = np.zeros((P, 2), np.float32); mkk[:64, 0] = 1; mkk[64:, 1] = 1
    m['mk'] = mkk
    order = (0, 1) if h == 0 else (1, 0)
    smu = inp['shift_mu'][0]
    mu = np.zeros((P, N_RW), np.float32)
    for j in range(24):
        mu[:, j] = smu[j * P:(j + 1) * P]
    for i, d in enumerate(order):
        mu[:96, T_XW + i] = smu[3072 + d * 96:3072 + (d + 1) * 96]
        mu[:96, T_XA + i] = smu[3072 + 192 + d * 96:3072 + 192 + (d + 1) * 96]
    for t in range(2):
        mu[:, T_XG + t] = smu[3072 + 384 + t * P:3072 + 384 + (t + 1) * P]
    m['mu'] = mu
    w2s = np.zeros((2, P, 1024), np.float32); a2s = np.zeros((2, P, 1024), np.float32)
    for i, d in enumerate(order):
        w2s[i, :96] = inp['rwkv_w2'][0, d]; a2s[i, :96] = inp['rwkv_a2'][0, d]
    m['w2s'] = w2s; m['a2s'] = a2s
    m['g2s'] = np.ascontiguousarray(inp['rwkv_g2'][0].reshape(2, P, 1024).transpose(1, 0, 2).reshape(P, 2048))
    chp = np.zeros((P, 8, 8), np.float32)
    def cl(v):
        return v.reshape(8, P).T
    chp[:, :, 0] = cl(inp['rwkv_w0'][0, order[0]]); chp[:, :, 1] = cl(inp['rwkv_w0'][0, order[1]])
    chp[:, :, 2] = cl(inp['rwkv_a0'][0, order[0]]); chp[:, :, 3] = cl(inp['rwkv_a0'][0, order[1]])
    chp[:, :, 4] = cl(inp['rwkv_k_k'][0]); chp[:, :, 5] = cl(inp['rwkv_k_a'][0]); chp[:, :, 6] = cl(inp['rwkv_r_k'][0].reshape(-1))
    m['chp'] = chp.reshape(P, 64)
    lnx = np.zeros((64, 16, 2), np.float32)
    lnx[:, :, 0] = inp['rwkv_lnx_g'][0].reshape(16, 64).T; lnx[:, :, 1] = inp['rwkv_lnx_b'][0].reshape(16, 64).T
    m['lnx'] = lnx.reshape(64, 32)
    ii = np.arange(64)
    su = (ii[:, None] < ii[None, :]).astype(np.float32); iu = (ii[:, None] <= ii[None, :]).astype(np.float32)
    mA = np.zeros((2, 64, 320), np.float32)
    mA[0] = np.concatenate([su, iu, su, iu, su.T], axis=1)
    mA[1] = np.concatenate([su.T, iu.T, su.T, iu.T, su], axis=1)
    m['maskA'] = mA
    rst = np.ones((P, 512), np.float32); rst[:, ::64] = 0
    m['rst'] = rst
    m['ones64'] = np.full((64, 64), 1.0 / 64, np.float32)
    m['wpa'] = np.ascontiguousarray(inp['w_pa'][0]); m['wpb'] = np.ascontiguousarray(inp['w_pb'][0])
    m['wout'] = np.ascontiguousarray(inp['w_out'][0])
    wrr = np.concatenate([inp['router_g_w'][0], inp['router_e_w'][0]], axis=1)
    m['wr'] = np.ascontiguousarray(wrr.reshape(KC, P, 36).transpose(1, 0, 2).reshape(P, KC * 36))
    m['rbias'] = np.ascontiguousarray(np.concatenate([inp['router_g_b'][0], inp['router_e_b'][0]])[None, :])
    if reuse is None:
        m['w1T'] = np.ascontiguousarray(inp['exp_w1'][0].reshape(NEXP, KC, P, DEXP).transpose(0, 2, 1, 3).reshape(NEXP, P, KC * DEXP))
        m['w3T'] = np.ascontiguousarray(inp['exp_w3'][0].reshape(NEXP, KC, P, DEXP).transpose(0, 2, 1, 3).reshape(NEXP, P, KC * DEXP))
        m['w2T'] = np.ascontiguousarray(inp['exp_w2'][0].reshape(NEXP, 4, P, D).transpose(0, 2, 1, 3).reshape(NEXP, P, 4 * D))
    else:
        m['w1T'] = reuse['w1T']; m['w3T'] = reuse['w3T']; m['w2T'] = reuse['w2T']
    return m


def kernel(**inp):
    inp = {k: np.asarray(v) for k, v in inp.items()}
    nc, stack = build()
    sh0 = shared_inputs(inp, 0)
    sh = [sh0, shared_inputs(inp, 1, reuse=sh0)]
    in_maps = []
    for core in range(8):
        m = dict(sh[core % 2])
        m.update(make_inputs(inp, core))
        in_maps.append(m)
    res = run_bass_kernel_spmd(nc, in_maps, core_ids=list(range(8)))
    outp = np.zeros((4, SEQ, D), np.float32)
    for core in range(8):
        b, h = core // 2, core % 2
        o = res.results[core]["out"]
        if h == 0:
            outp[b, :OWN] = o
        else:
            outp[b, OWN:] = o[::-1]
    return outp
```

```python
import numpy as np
from contextlib import ExitStack
import concourse.bass as bass
import concourse.mybir as mybir
from concourse.bass_utils import run_bass_kernel_spmd

F32 = mybir.dt.float32
BF16 = mybir.dt.bfloat16
I32 = mybir.dt.int32
ALU = mybir.AluOpType
AF = mybir.ActivationFunctionType
AX = mybir.AxisListType

P = 128
D = 2048
KC = 16
SEQ = 8192
CTX = 256
NTOK = SEQ + CTX
OWN = 4096
NEXP = 32
DEXP = 512

ENGS = ['pe', 'act', 'dve', 'pool', 'sp']
CWRAP = 30000
DK = 8
DWRAP = 3000


class Buf:
    __slots__ = ('w', 'r')

    def __init__(self):
        self.w = {}
        self.r = {}


def _evkey(ev):
    if ev[0] == 'c':
        return ('c', ev[1])
    return ('d', ev[1], ev[2] % DK)


class Sched:
    def __init__(self, nc, stack):
        self.nc = nc
        self.stack = stack
        self.ops = {e: [] for e in ENGS}
        self.cnt = {e: 0 for e in ENGS}
        self.dcnt = {e: 0 for e in ENGS}
        self.sems = {}
        self.waited = {e: {} for e in ENGS}
        self.last_dma = {}

    def sem(self, key):
        if key not in self.sems:
            name = "s_" + "_".join(str(k) for k in key)
            self.sems[key] = self.stack.enter_context(self.nc.semaphore(name))
        return self.sems[key]

    def _semval(self, ev):
        if ev[0] == 'c':
            return ('c', ev[1], ev[2] // CWRAP), ev[2] % CWRAP + 1
        n = ev[2]
        gen = n // DK
        return ('d', ev[1], n % DK, gen // DWRAP), 16 * (gen % DWRAP + 1)

    def op(self, eng, fn, reads=(), writes=(), dma=False):
        raw = {}
        oth = {}
        for b in reads:
            for k, v in b.w.items():
                if raw.get(k, -1) < v:
                    raw[k] = v
        for b in writes:
            for dd in (b.w, b.r):
                for k, v in dd.items():
                    if oth.get(k, -1) < v:
                        oth[k] = v
        if dma:
            n = self.dcnt[eng]
            self.dcnt[eng] += 1
            ev = ('d', eng, n)
            if n >= DK:
                k = ('d', eng, n % DK)
                if oth.get(k, -1) < n - DK:
                    oth[k] = n - DK
        else:
            idx = self.cnt[eng]
            self.cnt[eng] += 1
            ev = ('c', eng, idx)
        deps = {}
        for k, v in raw.items():
            if (not dma) and k == ('c', eng) and eng == 'pe':
                continue
            deps[k] = v
        for k, v in oth.items():
            if (not dma) and k == ('c', eng):
                continue
            if deps.get(k, -1) < v:
                deps[k] = v
        waits = []
        wd = self.waited[eng]
        for k, v in deps.items():
            e2 = ('c', k[1], v) if k[0] == 'c' else ('d', k[1], v)
            sk, sv = self._semval(e2)
            if wd.get(sk, 0) >= sv:
                continue
            wd[sk] = sv
            waits.append((self.sem(sk), sv))
        sk, sv = self._semval(ev)
        self.ops[eng].append((waits, fn, self.sem(sk), 16 if dma else 1))
        kk = _evkey(ev)
        for b in writes:
            b.w = {kk: ev[2]}
            b.r = {}
        for b in reads:
            if b.r.get(kk, -1) < ev[2]:
                b.r[kk] = ev[2]
        if dma:
            self.last_dma[kk] = ev[2]
        return ev

    def barrier(self):
        allb = Buf()
        for e in ENGS:
            if self.cnt[e] > 0:
                allb.w[('c', e)] = self.cnt[e] - 1
        for k, v in self.last_dma.items():
            allb.w[k] = v
        for e in ENGS:
            raw = dict(allb.w)
            waits = []
            wd = self.waited[e]
            for k, v in raw.items():
                if k == ('c', e):
                    continue
                e2 = ('c', k[1], v) if k[0] == 'c' else ('d', k[1], v)
                sk, sv = self._semval(e2)
                if wd.get(sk, 0) >= sv:
                    continue
                wd[sk] = sv
                waits.append((self.sem(sk), sv))
            if waits:
                idx = self.cnt[e]
                self.cnt[e] += 1
                sk, sv = self._semval(('c', e, idx))
                self.ops[e].append((waits, lambda en: en.nop(), self.sem(sk), 1))

    def emit(self):
        nc = self.nc
        with nc.Block() as blk:
            def replay(name):
                def f(en):
                    for waits, fn, sm, inc in self.ops[name]:
                        for (ws, wv) in waits:
                            en.wait_ge(ws, wv)
                        ins = fn(en)
                        ins.then_inc(sm, inc)
                return f
            blk.tensor(replay('pe'))
            blk.scalar(replay('act'))
            blk.vector(replay('dve'))
            blk.gpsimd(replay('pool'))
            blk.sync(replay('sp'))


class Arena:
    def __init__(self, nc, stack, name, nelem):
        self.t = stack.enter_context(nc.sbuf_tensor(name, [P, nelem], F32))
        self.n = nelem
        self.off = 0
        self.marks = []

    def alloc(self, *shape, dt=F32):
        n = int(np.prod(shape))
        n32 = n if dt == F32 else (n + 1) // 2
        assert self.off + n32 <= self.n, (self.off, n32, self.n)
        ap = self.t[:, self.off:self.off + n32]
        self.off += n32
        if dt != F32:
            ap = ap.bitcast(dt)
        if len(shape) == 2:
            ap = ap.rearrange("p (a b) -> p a b", a=shape[0])
        elif len(shape) == 3:
            ap = ap.rearrange("p (a b c) -> p a b c", a=shape[0], b=shape[1])
        return ap

    def mark(self):
        self.marks.append(self.off)

    def release(self):
        self.off = self.marks.pop()


class _B16:
    def __init__(self, ar):
        self.ar = ar

    def alloc(self, *shape):
        return self.ar.alloc(*shape, dt=BF16)

    def mark(self):
        pass

    def release(self):
        pass


T_R, T_K, T_V = 0, 8, 16
T_XW, T_XA, T_XG = 24, 26, 28
N_RW = 30
T_Q = 30
T_KK = 38
T_G = 46
N_FT = 78
PRW = NTOK + 4
PR_CTX = 1
PR_LAT = CTX + 3


def build(debug=None):
    nc = bass.Bass("TRN2", target_bir_lowering=False)
    stack = ExitStack()
    S = Sched(nc, stack)
    dbg_out = {}

    def din(name, shape, dt=F32):
        return nc.dram_tensor(name, list(shape), dt, kind="ExternalInput").ap()

    def dscr(name, shape, dt=F32):
        kind = "ExternalOutput" if (debug and name in debug) else "Internal"
        t = nc.dram_tensor(name, list(shape), dt, kind=kind).ap()
        return t

    x_loc = din("x_loc", [SEQ, D])
    ctx_loc = din("ctx_loc", [CTX, D])
    cT = din("cT", [P, KC * 2])
    adaT = din("adaT", [24, P, KC * 512])
    adab = din("adab", [1, 6 * D])
    n1g = din("n1g", [1, D])
    n2g = din("n2g", [1, D])
    winT = din("winT", [N_FT, P, KC * P])
    winV = din("winV", [8, P, KC * P])
    ident_in = din("ident", [P, P])
    bones_in = din("bones", [P, P])
    rotm_in = din("rotm", [P, P])
    cos_in = din("cosT", [P, SEQ])
    sin_in = din("sinT", [P, SEQ])
    qkg_in = din("qkg", [P, 2])
    dlam_in = din("dlam", [1, 256])
    subg_in = din("subg", [1, P])
    mk_in = din("mk", [P, 2])
    mu_in = din("mu", [P, N_RW])
    w2s_in = din("w2s", [2, P, 1024])
    a2s_in = din("a2s", [2, P, 1024])
    g2s_in = din("g2s", [P, 2 * 1024])
    chp_in = din("chp", [P, 8 * 8])
    lnx_in = din("lnx", [64, 16 * 2])
    maskA_in = din("maskA", [2, 64, 320])
    rst_in = din("rst", [P, 512])
    ones64_in = din("ones64", [64, 64])
    wpa_in = din("wpa", [1024, D])
    wpb_in = din("wpb", [1024, D])
    wout_in = din("wout", [D, D])
    wr_in = din("wr", [P, KC * 36])
    rbias_in = din("rbias", [1, 36])
    w1T_in = din("w1T", [NEXP, P, KC * DEXP])
    w3T_in = din("w3T", [NEXP, P, KC * DEXP])
    w2T_in = din("w2T", [NEXP, P, 4 * D])
    out = nc.dram_tensor("out", [OWN, D], F32, kind="ExternalOutput").ap()

    MODS = dscr("MODS", [12, D])
    PR = dscr("PR", [N_RW, P, PRW])
    QT = dscr("QT", [8, P, OWN])
    KT = dscr("KT", [8, P, NTOK])
    VV = dscr("VV", [NTOK, 1024])
    GT = dscr("GT", [32, P, OWN])
    YD = dscr("YD", [8, P, OWN], BF16)
    NCH = NTOK // 64
    KBd = dscr("KBd", [2, 1024, NCH * 128])
    KRd = dscr("KRd", [2, 1024, NCH * 128])
    VSd = dscr("VSd", [1024, NCH * 64])
    GCd = dscr("GCd", [2, 1024, NCH])
    BON = dscr("BON", [1024, OWN])
    GGd = dscr("GGd", [1024, OWN])
    YR = dscr("YR", [1024, OWN], BF16)
    MX = dscr("MX", [16, P, OWN], BF16)
    XN = dscr("XN", [OWN, D])
    H2T = dscr("H2T", [KC, P, OWN], BF16)
    GMT = dscr("GMT", [NEXP, OWN])
    W1B = dscr("W1B", [NEXP, P, KC * DEXP], BF16)
    W3B = dscr("W3B", [NEXP, P, KC * DEXP], BF16)
    W2B = dscr("W2B", [NEXP, P, 4 * D], BF16)

    AF32 = Arena(nc, stack, "arena", 49152)
    AB16 = _B16(AF32)
    ps = [stack.enter_context(nc.psum_tensor(f"ps{i}", [P, 512], F32)) for i in range(8)]
    psb = [ps[6][:, :].bitcast(BF16), ps[7][:, :].bitcast(BF16)]
    bps = [Buf() for _ in range(8)]
    bpsb = [bps[6], bps[7]]

    def mm(out_, lhsT, rhs, reads, writes, start=True, stop=True):
        S.op('pe', lambda e: e.matmul(out_, lhsT=lhsT, rhs=rhs, start=start, stop=stop), reads=reads, writes=writes)

    def tr(out_, in_, idn, reads, writes):
        S.op('pe', lambda e: e.transpose(out=out_, in_=in_, identity=idn), reads=reads, writes=writes)

    def tt(eng, out_, in0, in1, op, reads, writes):
        S.op(eng, lambda e: e.tensor_tensor(out=out_, in0=in0, in1=in1, op=op), reads=reads, writes=writes)

    def ts(eng, out_, in0, s1, s2, op0, op1, reads, writes):
        if s2 is None:
            S.op(eng, lambda e: e.tensor_scalar(out=out_, in0=in0, scalar1=s1, scalar2=None, op0=op0), reads=reads, writes=writes)
        else:
            S.op(eng, lambda e: e.tensor_scalar(out=out_, in0=in0, scalar1=s1, scalar2=s2, op0=op0, op1=op1), reads=reads, writes=writes)

    def stt(eng, out_, in0, sc, in1, op0, op1, reads, writes):
        S.op(eng, lambda e: e.scalar_tensor_tensor(out=out_, in0=in0, scalar=sc, in1=in1, op0=op0, op1=op1), reads=reads, writes=writes)

    def act(out_, in_, func, reads, writes, scale=1.0, bias=0.0, accum_out=None):
        if accum_out is None:
            S.op('act', lambda e: e.activation(out=out_, in_=in_, func=func, scale=scale, bias=bias), reads=reads, writes=writes)
        else:
            S.op('act', lambda e: e.activation(out=out_, in_=in_, func=func, scale=scale, bias=bias, accum_out=accum_out), reads=reads, writes=writes)

    def cp(eng, out_, in_, reads, writes):
        if eng == 'act':
            S.op(eng, lambda e: e.activation(out=out_, in_=in_, func=AF.Copy), reads=reads, writes=writes)
        else:
            S.op(eng, lambda e: e.tensor_copy(out=out_, in_=in_), reads=reads, writes=writes)

    def rcp(out_, in_, reads, writes):
        S.op('dve', lambda e: e.reciprocal(out=out_, in_=in_), reads=reads, writes=writes)

    def dma(out_, in_, reads=(), writes=None, slow=False):
        w = [Buf()] if writes is None else writes
        if slow:
            S.op('sp', lambda e: e.dma_start(out=out_, in_=in_, allow_slow_non_contiguous=True), reads=reads, writes=w, dma=True)
        else:
            S.op('sp', lambda e: e.dma_start(out=out_, in_=in_), reads=reads, writes=w, dma=True)

    ident = AF32.alloc(P)
    b_ident = Buf()
    identb = AB16.alloc(P)
    b_identb = Buf()
    S.op('sp', lambda e: e.dma_start(out=ident, in_=ident_in), writes=[b_ident], dma=True)
    S.op('dve', lambda e: e.tensor_copy(out=identb, in_=ident), reads=[b_ident], writes=[b_identb])

    AF32.mark(); AB16.mark()
    cs = AF32.alloc(KC, 2)
    b_cs = Buf()
    S.op('sp', lambda e: e.dma_start(out=cs, in_=cT.rearrange("p (a b) -> p a b", a=KC)), writes=[b_cs], dma=True)
    S.op('act', lambda e: e.activation(out=cs, in_=cs, func=AF.Silu), reads=[b_cs], writes=[b_cs])
    modrows = AF32.alloc(6 * D)
    b_mod = Buf()
    brow = AF32.alloc(6 * D)
    b_brow = Buf()
    for r in range(2):
        S.op('sp', (lambda r: lambda e: e.dma_start(out=brow[r:r + 1, :], in_=adab))(r), writes=[b_brow], dma=True)
    wA = [AF32.alloc(KC, 512) for _ in range(2)]
    b_wA = [Buf(), Buf()]
    for j in range(24):
        w = wA[j % 2]; bw = b_wA[j % 2]
        S.op('sp', (lambda w, j: lambda e: e.dma_start(out=w, in_=adaT[j].rearrange("p (a b) -> p a b", a=KC)))(w, j),
             writes=[bw], dma=True)
        pb = j % 2
        for kc in range(KC):
            S.op('pe', (lambda w, kc, pb: lambda e: e.matmul(ps[pb][0:2, :], lhsT=cs[:, kc, :], rhs=w[:, kc, :],
                                                              start=(kc == 0), stop=(kc == KC - 1)))(w, kc, pb),
                 reads=[b_cs, bw], writes=[bps[pb]])
        S.op('dve', (lambda j, pb: lambda e: e.tensor_tensor(out=modrows[0:2, j * 512:(j + 1) * 512], in0=ps[pb][0:2, :],
                                                             in1=brow[0:2, j * 512:(j + 1) * 512], op=ALU.add))(j, pb),
             reads=[bps[pb], b_brow], writes=[b_mod])
    for r in range(2):
        S.op('sp', (lambda r: lambda e: e.dma_start(out=brow[r:r + 1, 0:D], in_=n1g))(r), writes=[b_brow], dma=True)
        S.op('sp', (lambda r: lambda e: e.dma_start(out=brow[r:r + 1, D:2 * D], in_=n2g))(r), writes=[b_brow], dma=True)
    S.op('dve', lambda e: e.scalar_tensor_tensor(out=modrows[0:2, D:2 * D], in0=modrows[0:2, D:2 * D], scalar=1.0,
                                                 in1=brow[0:2, 0:D], op0=ALU.add, op1=ALU.mult),
         reads=[b_mod, b_brow], writes=[b_mod])
    S.op('dve', lambda e: e.scalar_tensor_tensor(out=modrows[0:2, 4 * D:5 * D], in0=modrows[0:2, 4 * D:5 * D], scalar=1.0,
                                                 in1=brow[0:2, D:2 * D], op0=ALU.add, op1=ALU.mult),
         reads=[b_mod, b_brow], writes=[b_mod])
    b_MODS = Buf()
    for (mi, col) in ((0, 1), (2, 0), (4, 4), (6, 3), (8, 2), (10, 5)):
        S.op('sp', (lambda mi, col: lambda e: e.dma_start(out=MODS[mi:mi + 2, :], in_=modrows[0:2, col * D:(col + 1) * D]))(mi, col),
             reads=[b_mod], writes=[b_MODS], dma=True)
    S.barrier()
    AF32.release(); AB16.release()

    if debug and debug.get('stop') == 'A':
        return finish(nc, S, stack)

    AF32.mark(); AB16.mark()
    Abc = [AF32.alloc(D) for _ in range(2)]
    Bbc = [AF32.alloc(D) for _ in range(2)]
    b_AB = Buf()
    for r in range(2):
        S.op('sp', (lambda r: lambda e: e.dma_start(out=Abc[r], in_=MODS[r:r + 1, :].partition_broadcast(P)))(r),
             reads=[b_MODS], writes=[b_AB], dma=True)
        S.op('sp', (lambda r: lambda e: e.dma_start(out=Bbc[r], in_=MODS[2 + r:3 + r, :].partition_broadcast(P)))(r),
             reads=[b_MODS], writes=[b_AB], dma=True)
    TS = 2048
    xt = [AF32.alloc(D) for _ in range(2)]; b_xt = [Buf(), Buf()]
    hxb = [AB16.alloc(D) for _ in range(2)]; b_hxb = [Buf(), Buf()]
    junk = AB16.alloc(D); b_junk = Buf()
    stat = [AF32.alloc(2) for _ in range(2)]; b_stat = [Buf(), Buf()]
    hT = AB16.alloc(KC, TS); b_hT = Buf()
    wf = [AF32.alloc(KC, P) for _ in range(2)]; b_wf = [Buf(), Buf()]
    wb = [AB16.alloc(KC, P) for _ in range(2)]; b_wb = [Buf(), Buf()]
    stg = [AF32.alloc(512) for _ in range(3)]; b_stg = [Buf() for _ in range(3)]
    zero = AF32.alloc(4); b_zero = Buf()
    S.op('dve', lambda e: e.memset(zero, 0.0), writes=[b_zero])
    b_PR = Buf(); b_QT = Buf(); b_KT = Buf(); b_VV = Buf(); b_GT = Buf()
    for j in range(N_RW):
        for c in (0, PR_CTX + CTX, PR_CTX + CTX + 1, PRW - 1):
            S.op('sp', (lambda j, c: lambda e: e.dma_start(out=PR[j, :, c:c + 1], in_=zero[:, 0:1], allow_slow_non_contiguous=True))(j, c),
                 reads=[b_zero], writes=[Buf()], dma=True)

    cnt = {'x': 0, 'w': 0, 'ps': 0, 'stg': 0, 'pb': 0}

    supers = [(ctx_loc, 0, CTX, 1, PR_CTX, 0, False)]
    for s4 in range(SEQ // TS):
        supers.append((x_loc, s4 * TS, TS, 0, PR_LAT + s4 * TS, CTX + s4 * TS, s4 * TS < OWN))
    if debug and debug.get('nsuper'):
        supers = supers[:debug['nsuper']]
    def do_super(src, row0, ntok, kind, prc0, kt0, own):
        for tt in range(ntok // P):
            i = cnt['x'] % 2; cnt['x'] += 1
            X = xt[i]; bX = b_xt[i]; H = hxb[i]; bH = b_hxb[i]; st = stat[i]; bst = b_stat[i]
            S.op('sp', (lambda X, r0: lambda e: e.dma_start(out=X, in_=src[r0:r0 + P, :]))(X, row0 + tt * P),
                 writes=[bX], dma=True)
            S.op('act', (lambda X, st: lambda e: e.activation(out=junk, in_=X, func=AF.Square, accum_out=st[:, 0:1]))(X, st),
                 reads=[bX], writes=[b_junk, bst])
            S.op('act', (lambda st: lambda e: e.activation(out=st[:, 1:2], in_=st[:, 0:1], func=AF.Sqrt, scale=1.0 / D, bias=1e-6))(st),
                 reads=[bst], writes=[bst])
            S.op('dve', (lambda st: lambda e: e.reciprocal(out=st[:, 1:2], in_=st[:, 1:2]))(st), reads=[bst], writes=[bst])
            S.op('dve', (lambda X, st: lambda e: e.scalar_tensor_tensor(out=X, in0=X, scalar=st[:, 1:2], in1=Abc[kind],
                                                                        op0=ALU.mult, op1=ALU.mult))(X, st),
                 reads=[bX, bst, b_AB], writes=[bX])
            S.op('dve', (lambda X, H: lambda e: e.tensor_tensor(out=H, in0=X, in1=Bbc[kind], op=ALU.add))(X, H),
                 reads=[bX, b_AB], writes=[bH])
            for half in range(2):
                pb = cnt['pb'] % 2; cnt['pb'] += 1
                for q in range(8):
                    kc = half * 8 + q
                    S.op('pe', (lambda H, kc, q, pb: lambda e: e.transpose(out=psb[pb][:, q * P:(q + 1) * P],
                                                                          in_=H[:, kc * P:(kc + 1) * P], identity=identb))(H, kc, q, pb),
                         reads=[bH, b_identb], writes=[bpsb[pb]])
                S.op('act' if half == 0 else 'dve',
                     (lambda half, pb, tt: lambda e: (e.activation(out=hT[:, half * 8:half * 8 + 8, tt * P:(tt + 1) * P],
                                                                   in_=psb[pb].rearrange("p (a b) -> p a b", a=8), func=AF.Copy)
                                                      if half == 0 else
                                                      e.tensor_copy(out=hT[:, half * 8:half * 8 + 8, tt * P:(tt + 1) * P],
                                                                    in_=psb[pb].rearrange("p (a b) -> p a b", a=8))))(half, pb, tt),
                     reads=[bpsb[pb]], writes=[b_hT])
        tiles = list(range(N_RW)) + list(range(T_KK, T_KK + 8))
        if own:
            tiles += list(range(T_Q, T_Q + 8)) + list(range(T_G, T_G + 32))
        if debug and debug.get('ntiles'):
            tiles = tiles[:debug['ntiles']]
        if debug and debug.get('vonly'):
            tiles = []
        nch = max(1, ntok // 512)
        cw = min(512, ntok)
        for j in tiles:
            i = cnt['w'] % 2; cnt['w'] += 1
            S.op('sp', (lambda i, j: lambda e: e.dma_start(out=wf[i], in_=winT[j].rearrange("p (a b) -> p a b", a=KC)))(i, j),
                 writes=[b_wf[i]], dma=True)
            S.op('pool', (lambda i: lambda e: e.tensor_copy(out=wb[i], in_=wf[i]))(i), reads=[b_wf[i]], writes=[b_wb[i]])
            for ch in range(nch):
                pi = cnt['ps'] % 4; cnt['ps'] += 1
                for kc in range(KC):
                    S.op('pe', (lambda i, kc, pi, ch: lambda e: e.matmul(ps[pi][:, 0:cw], lhsT=wb[i][:, kc, :],
                                                                          rhs=hT[:, kc, ch * 512:ch * 512 + cw],
                                                                          start=(kc == 0), stop=(kc == KC - 1)))(i, kc, pi, ch),
                         reads=[b_wb[i], b_hT], writes=[bps[pi]])
                si = cnt['stg'] % 3; cnt['stg'] += 1
                isg = j >= T_G
                S.op('act', (lambda pi, si, isg: lambda e: e.activation(out=stg[si][:, 0:cw], in_=ps[pi][:, 0:cw],
                                                                         func=(AF.Sigmoid if isg else AF.Copy)))(pi, si, isg),
                     reads=[bps[pi]], writes=[b_stg[si]])
                if j < N_RW:
                    dst = PR[j, :, prc0 + ch * 512:prc0 + ch * 512 + cw]; bd = b_PR
                elif j < T_KK:
                    dst = QT[j - T_Q, :, row0 + ch * 512:row0 + ch * 512 + cw]; bd = b_QT
                elif j < T_G:
                    dst = KT[j - T_KK, :, kt0 + ch * 512:kt0 + ch * 512 + cw]; bd = b_KT
                else:
                    dst = GT[j - T_G, :, row0 + ch * 512:row0 + ch * 512 + cw]; bd = b_GT
                S.op('sp', (lambda dst, si: lambda e: e.dma_start(out=dst, in_=stg[si][:, 0:cw]))(dst, si),
                     reads=[b_stg[si]], writes=[Buf()], dma=True)
        if not (debug and debug.get('ntiles')):
            for vj in range(8):
                i = cnt['w'] % 2; cnt['w'] += 1
                S.op('sp', (lambda i, vj: lambda e: e.dma_start(out=wf[i], in_=winV[vj].rearrange("p (a b) -> p a b", a=KC)))(i, vj),
                     writes=[b_wf[i]], dma=True)
                S.op('pool', (lambda i: lambda e: e.tensor_copy(out=wb[i], in_=wf[i]))(i), reads=[b_wf[i]], writes=[b_wb[i]])
                for t4 in range(0, ntok // P, 4):
                    n4 = min(4, ntok // P - t4)
                    pi = cnt['ps'] % 4; cnt['ps'] += 1
                    for q in range(n4):
                        for kc in range(KC):
                            S.op('pe', (lambda i, kc, pi, q, t4: lambda e: e.matmul(ps[pi][:, q * P:(q + 1) * P],
                                                                                    lhsT=hT[:, kc, (t4 + q) * P:(t4 + q + 1) * P],
                                                                                    rhs=wb[i][:, kc, :],
                                                                                    start=(kc == 0), stop=(kc == KC - 1)))(i, kc, pi, q, t4),
                                 reads=[b_wb[i], b_hT], writes=[bps[pi]])
                    si = cnt['stg'] % 3; cnt['stg'] += 1
                    S.op('act', (lambda pi, si, n4: lambda e: e.activation(out=stg[si][:, 0:n4 * P], in_=ps[pi][:, 0:n4 * P], func=AF.Copy))(pi, si, n4),
                         reads=[bps[pi]], writes=[b_stg[si]])
                    dst = VV[kt0 + t4 * P:kt0 + (t4 + n4) * P, vj * P:(vj + 1) * P].rearrange("(q p) c -> p q c", p=P)
                    S.op('sp', (lambda dst, si, n4: lambda e: e.dma_start(out=dst, in_=stg[si][:, 0:n4 * P].rearrange("p (q c) -> p q c", q=n4)))(dst, si, n4),
                         reads=[b_stg[si]], writes=[Buf()], dma=True)
    for sp_ in supers:
        do_super(*sp_)
    S.barrier()
    AF32.release(); AB16.release()
    if debug and debug.get('stop') == 'B':
        return finish(nc, S, stack)


    AF32.mark()
    bones = AF32.alloc(P); rotm = AF32.alloc(P); qkg = AF32.alloc(2); b_cF = Buf()
    S.op('sp', lambda e: e.dma_start(out=bones, in_=bones_in), writes=[b_cF], dma=True)
    S.op('sp', lambda e: e.dma_start(out=rotm, in_=rotm_in), writes=[b_cF], dma=True)
    S.op('sp', lambda e: e.dma_start(out=qkg, in_=qkg_in), writes=[b_cF], dma=True)
    dl = AF32.alloc(256); dl2 = AF32.alloc(2, 64); lam = AF32.alloc(4); subg = AF32.alloc(P); b_lam = Buf()
    S.op('sp', lambda e: e.dma_start(out=dl, in_=dlam_in.partition_broadcast(P)), writes=[b_lam], dma=True)
    S.op('sp', lambda e: e.dma_start(out=subg, in_=subg_in.partition_broadcast(P)), writes=[b_lam], dma=True)
    dlv = dl.rearrange("p (a b c) -> p a b c", a=2, b=2)
    S.op('dve', lambda e: e.tensor_tensor(out=dl2, in0=dlv[:, :, 0, :], in1=dlv[:, :, 1, :], op=ALU.mult), reads=[b_lam], writes=[b_lam])
    S.op('dve', lambda e: e.tensor_reduce(out=lam[:, 0:2], in_=dl2, axis=AX.X, op=ALU.add), reads=[b_lam], writes=[b_lam])
    S.op('act', lambda e: e.activation(out=lam[:, 0:2], in_=lam[:, 0:2], func=AF.Exp), reads=[b_lam], writes=[b_lam])
    S.op('dve', lambda e: e.scalar_tensor_tensor(out=lam[:, 2:3], in0=lam[:, 1:2], scalar=-0.2, in1=lam[:, 0:1], op0=ALU.add, op1=ALU.subtract),
         reads=[b_lam], writes=[b_lam])
    S.op('dve', lambda e: e.tensor_scalar(out=subg, in0=subg, scalar1=0.8, scalar2=None, op0=ALU.mult), reads=[b_lam], writes=[b_lam])
    Kb = AF32.alloc(2, NTOK, dt=BF16); b_Kb = Buf()
    kfull = AF32.alloc(512, dt=BF16); b_kfull = Buf()
    mk = AF32.alloc(2);
    S.op('sp', lambda e: e.dma_start(out=mk, in_=mk_in), writes=[b_cF], dma=True)
    Vb = AF32.alloc(NTOK // P, 132, dt=BF16); b_Vb = Buf()
    S.op('pool', lambda e: e.memset(Vb[:, :, 128:129], 1.0), writes=[b_Vb])
    tin = [AF32.alloc(512) for _ in range(2)]; b_tin = [Buf(), Buf()]
    tcs = [AF32.alloc(2, 512) for _ in range(2)]; b_tcs = [Buf(), Buf()]
    tsq = AF32.alloc(512); b_tsq = Buf()
    trs = AF32.alloc(512); b_trs = Buf()
    tkn = AF32.alloc(512); b_tkn = Buf()
    Qb = [AF32.alloc(512, dt=BF16) for _ in range(2)]; b_Qb = [Buf(), Buf()]
    PT = [AF32.alloc(512, dt=BF16) for _ in range(3)]; b_PT = [Buf() for _ in range(3)]
    vin = [AF32.alloc(P) for _ in range(2)]; b_vin = [Buf(), Buf()]
    ot = [AF32.alloc(2, 132) for _ in range(2)]; b_ot = [Buf(), Buf()]
    osm = [AF32.alloc(8) for _ in range(2)]
    yb = [AF32.alloc(P, dt=BF16) for _ in range(2)]; b_yb = [Buf(), Buf()]
    ydt = [AF32.alloc(512, dt=BF16) for _ in range(2)]; b_ydt = [Buf(), Buf()]
    cF = {'t': 0, 'q': 0, 'pt': 0, 'v': 0, 'o': 0, 'sc': 0, 'yd': 0}

    def qk_prep(srcap, n, dst, b_dst, gcol, tab0):
        i = cF['t'] % 2; cF['t'] += 1
        T = tin[i]; bT = b_tin[i]; CS = tcs[i]; bCS = b_tcs[i]
        S.op('sp', lambda e: e.dma_start(out=T[:, 0:n], in_=srcap), writes=[bT], dma=True)
        if tab0 is not None:
            S.op('sp', lambda e: e.dma_start(out=CS[:, 0, 0:n], in_=cos_in[:, tab0:tab0 + n]), writes=[bCS], dma=True)
            S.op('sp', lambda e: e.dma_start(out=CS[:, 1, 0:n], in_=sin_in[:, tab0:tab0 + n]), writes=[bCS], dma=True)
        S.op('act', lambda e: e.activation(out=tsq[:, 0:n], in_=T[:, 0:n], func=AF.Square), reads=[bT], writes=[b_tsq])
        S.op('pe', lambda e: e.matmul(ps[4][:, 0:n], lhsT=bones, rhs=tsq[:, 0:n], start=True, stop=True), reads=[b_tsq, b_cF], writes=[bps[4]])
        S.op('act', lambda e: e.activation(out=trs[:, 0:n], in_=ps[4][:, 0:n], func=AF.Sqrt, scale=1.0 / 64, bias=1e-6), reads=[bps[4]], writes=[b_trs])
        S.op('dve', lambda e: e.reciprocal(out=trs[:, 0:n], in_=trs[:, 0:n]), reads=[b_trs], writes=[b_trs])
        if tab0 is None:
            S.op('dve', lambda e: e.scalar_tensor_tensor(out=dst, in0=T[:, 0:n], scalar=qkg[:, gcol:gcol + 1], in1=trs[:, 0:n], op0=ALU.mult, op1=ALU.mult),
                 reads=[bT, b_trs, b_cF], writes=[b_dst])
            return
        S.op('dve', lambda e: e.scalar_tensor_tensor(out=tkn[:, 0:n], in0=T[:, 0:n], scalar=qkg[:, gcol:gcol + 1], in1=trs[:, 0:n], op0=ALU.mult, op1=ALU.mult),
             reads=[bT, b_trs, b_cF], writes=[b_tkn])
        S.op('pe', lambda e: e.matmul(ps[5][:, 0:n], lhsT=rotm, rhs=tkn[:, 0:n], start=True, stop=True), reads=[b_tkn, b_cF], writes=[bps[5]])
        S.op('dve', lambda e: e.tensor_tensor(out=CS[:, 1, 0:n], in0=ps[5][:, 0:n], in1=CS[:, 1, 0:n], op=ALU.mult), reads=[bps[5], bCS], writes=[bCS])
        S.op('pool', lambda e: e.tensor_tensor(out=CS[:, 0, 0:n], in0=tkn[:, 0:n], in1=CS[:, 0, 0:n], op=ALU.mult), reads=[b_tkn, bCS], writes=[bCS])
        S.op('dve', lambda e: e.tensor_tensor(out=dst, in0=CS[:, 0, 0:n], in1=CS[:, 1, 0:n], op=ALU.add), reads=[bCS], writes=[b_dst])

    def attn_head(h):
        def ksplit(c0, n):
            for m in range(2):
                S.op('dve' if m == 0 else 'pool', (lambda m: lambda e: e.tensor_scalar(out=Kb[:, m, c0:c0 + n], in0=kfull[:, 0:n], scalar1=mk[:, m:m + 1],
                                                                                 scalar2=None, op0=ALU.mult))(m),
                     reads=[b_kfull, b_cF], writes=[b_Kb])
        qk_prep(KT[h, :, 0:CTX], CTX, kfull[:, 0:CTX], b_kfull, 1, None)
        ksplit(0, CTX)
        for c in range(SEQ // 512):
            qk_prep(KT[h, :, CTX + c * 512:CTX + (c + 1) * 512], 512, kfull, b_kfull, 1, c * 512)
            ksplit(CTX + c * 512, 512)
        for t in range(NTOK // P):
            i = cF['v'] % 2; cF['v'] += 1
            S.op('sp', (lambda i, t: lambda e: e.dma_start(out=vin[i], in_=VV[t * P:(t + 1) * P, h * P:(h + 1) * P]))(i, t), writes=[b_vin[i]], dma=True)
            S.op('pool', (lambda i, t: lambda e: e.tensor_copy(out=Vb[:, t, 0:P], in_=vin[i]))(i, t), reads=[b_vin[i]], writes=[b_Vb])
        QC = 256
        nq = OWN // QC
        NKT = (debug or {}).get('nkt') or NTOK // P
        if debug and debug.get('nq'):
            nq = debug['nq']
        for qc in range(nq):
            qi = cF['q'] % 2; cF['q'] += 1
            qk_prep(QT[h, :, qc * QC:(qc + 1) * QC], QC, Qb[qi][:, 0:QC], b_Qb[qi], 0, qc * QC)
            for kt in range(NKT):
                for m in range(2):
                    sc = cF['sc'] % 2; cF['sc'] += 1
                    S.op('pe', (lambda m, kt, qi, sc: lambda e: e.matmul(ps[4 + sc][:, 0:QC], lhsT=Kb[:, m, kt * P:(kt + 1) * P],
                                                                        rhs=Qb[qi][:, 0:QC], start=True, stop=True))(m, kt, qi, sc),
                         reads=[b_Kb, b_Qb[qi]], writes=[bps[4 + sc]])
                    pi = cF['pt'] % 3; cF['pt'] += 1
                    S.op('act', (lambda sc, pi: lambda e: e.activation(out=PT[pi][:, 0:QC], in_=ps[4 + sc][:, 0:QC], func=AF.Exp, scale=0.125))(sc, pi),
                         reads=[bps[4 + sc]], writes=[b_PT[pi]])
                    for qs in range(2):
                        S.op('pe', (lambda m, kt, pi, qs: lambda e: e.matmul(ps[qs * 2 + m][:, 0:129], lhsT=PT[pi][:, qs * P:(qs + 1) * P],
                                                                            rhs=Vb[:, kt, 0:129], start=(kt == 0), stop=(kt == NKT - 1)))(m, kt, pi, qs),
                             reads=[b_PT[pi], b_Vb], writes=[bps[qs * 2 + m]])
            yi = cF['yd'] % 2; cF['yd'] += 1
            for qs in range(2):
                oi = cF['o'] % 2; cF['o'] += 1
                O = ot[oi]; bO = b_ot[oi]; sm = osm[oi]
                for m in range(2):
                    S.op('dve', (lambda O, qs, m: lambda e: e.tensor_copy(out=O[:, m, 0:129], in_=ps[qs * 2 + m][:, 0:129]))(O, qs, m),
                         reads=[bps[qs * 2 + m]], writes=[bO])
                S.op('dve', (lambda O, sm: lambda e: e.reciprocal(out=sm[:, 0:2], in_=O[:, :, 128]))(O, sm), reads=[bO], writes=[bO])
                S.op('dve', (lambda sm: lambda e: e.tensor_tensor(out=sm[:, 1:2], in0=sm[:, 1:2], in1=lam[:, 2:3], op=ALU.mult))(sm), reads=[bO, b_lam], writes=[bO])
                S.op('dve', (lambda O, sm: lambda e: e.tensor_scalar(out=O[:, 0, 0:P], in0=O[:, 0, 0:P], scalar1=sm[:, 0:1], scalar2=None, op0=ALU.mult))(O, sm),
                     reads=[bO], writes=[bO])
                S.op('dve', (lambda O, sm: lambda e: e.scalar_tensor_tensor(out=O[:, 0, 0:P], in0=O[:, 1, 0:P], scalar=sm[:, 1:2], in1=O[:, 0, 0:P],
                                                                            op0=ALU.mult, op1=ALU.add))(O, sm), reads=[bO], writes=[bO])
                S.op('act', (lambda O, sm: lambda e: e.activation(out=O[:, 1, 0:P], in_=O[:, 0, 0:P], func=AF.Square, accum_out=sm[:, 2:3]))(O, sm),
                     reads=[bO], writes=[bO])
                S.op('act', (lambda sm: lambda e: e.activation(out=sm[:, 3:4], in_=sm[:, 2:3], func=AF.Sqrt, scale=1.0 / P, bias=1e-5))(sm), reads=[bO], writes=[bO])
                S.op('dve', (lambda sm: lambda e: e.reciprocal(out=sm[:, 3:4], in_=sm[:, 3:4]))(sm), reads=[bO], writes=[bO])
                S.op('dve', (lambda O, sm, oi: lambda e: e.scalar_tensor_tensor(out=yb[oi], in0=O[:, 0, 0:P], scalar=sm[:, 3:4], in1=subg,
                                                                                op0=ALU.mult, op1=ALU.mult))(O, sm, oi),
                     reads=[bO, b_lam], writes=[b_yb[oi]])
                pb = cnt['pb'] % 2; cnt['pb'] += 1
                S.op('pe', (lambda oi, pb: lambda e: e.transpose(out=psb[pb][:, 0:P], in_=yb[oi], identity=identb))(oi, pb),
                     reads=[b_yb[oi], b_identb], writes=[bpsb[pb]])
                S.op('act', (lambda pb, yi, qs: lambda e: e.activation(out=ydt[yi][:, qs * P:(qs + 1) * P], in_=psb[pb][:, 0:P], func=AF.Copy))(pb, yi, qs),
                     reads=[bpsb[pb]], writes=[b_ydt[yi]])
            S.op('sp', (lambda yi, qc: lambda e: e.dma_start(out=YD[h, :, qc * QC:(qc + 1) * QC], in_=ydt[yi][:, 0:QC]))(yi, qc),
                 reads=[b_ydt[yi]], writes=[Buf()], dma=True)

    nheads = 8
    if debug and debug.get('nheads'):
        nheads = debug['nheads']
    if debug and debug.get('skipF'):
        nheads = 0
    for h in range(nheads):
        attn_head(h)
    S.barrier()
    AF32.release()
    if debug and debug.get('stop') == 'F':
        return finish(nc, S, stack)

    C0 = 0.6065306597126334
    if not (debug and debug.get('skipC')):
        AF32.mark()
        mu = AF32.alloc(N_RW); chp = AF32.alloc(8, 8); bonesC = AF32.alloc(P); rstm = AF32.alloc(512)
        w2s = AF32.alloc(2, 1024); a2s = AF32.alloc(2, 1024); g2s = AF32.alloc(2, 1024)
        omka = AF32.alloc(8)
        b_cC = Buf()
        dma(mu, mu_in, writes=[b_cC]); dma(chp, chp_in.rearrange("p (a b) -> p a b", a=8), writes=[b_cC])
        dma(bonesC, bones_in, writes=[b_cC]); dma(rstm, rst_in, writes=[b_cC])
        dma(w2s, w2s_in.rearrange("d p c -> p d c"), writes=[b_cC]); dma(a2s, a2s_in.rearrange("d p c -> p d c"), writes=[b_cC])
        dma(g2s, g2s_in.rearrange("p (a b) -> p a b", a=2), writes=[b_cC])
        ts('dve', omka, chp[:, :, 5], -1.0, 1.0, ALU.mult, ALU.add, [b_cC], [b_cC])
        NB = 514
        raw = [AF32.alloc(NB) for _ in range(3)]; b_raw = [Buf() for _ in range(3)]
        tmpS = [AF32.alloc(512) for _ in range(2)]; b_tmpS = [Buf(), Buf()]
        lx = [AF32.alloc(512) for _ in range(6)]; b_lx = [Buf() for _ in range(6)]
        rr = AF32.alloc(512); kk_ = AF32.alloc(512); vv_ = AF32.alloc(512); kn_ = AF32.alloc(512)
        b_rr = Buf(); b_k = Buf(); b_v = Buf(); b_kn = Buf()
        sg = [AF32.alloc(512) for _ in range(2)]; aa = [AF32.alloc(512) for _ in range(2)]
        b_sg = [Buf(), Buf()]; b_aa = [Buf(), Buf()]
        kd = [AF32.alloc(512) for _ in range(2)]; b_kd = [Buf(), Buf()]
        bb = AF32.alloc(512); b_bb = Buf()
        csg = AF32.alloc(512); b_csg = Buf()
        cpv = AF32.alloc(512); b_cpv = Buf()
        gm = AF32.alloc(512); gp = AF32.alloc(512); gq = AF32.alloc(512); b_gm = Buf(); b_gp = Buf(); b_gq = Buf()
        gcs = AF32.alloc(8); b_gcs = Buf()
        o4 = [AF32.alloc(512) for _ in range(4)]; b_o4 = [Buf() for _ in range(4)]
        t1 = AF32.alloc(512); b_t1 = Buf()
        gst = AF32.alloc(512); b_gst = Buf()
        cC = {'raw': 0, 'tmp': 0, 'o': 0}

        def load_shift(j, pc0, n, dst, b_dst, post=None):
            i = cC['raw'] % 3; cC['raw'] += 1
            R = raw[i]; bR = b_raw[i]
            ti = cC['tmp'] % 2; cC['tmp'] += 1
            T = tmpS[ti]; bT = b_tmpS[ti]
            dma(R[:, 0:n + 2], PR[j, :, pc0 - 1:pc0 + n + 1], writes=[bR])
            tt('pool', T[:, 0:n], R[:, 0:n], R[:, 2:n + 2], ALU.add, [bR], [bT])
            stt('dve', T[:, 0:n], T[:, 0:n], 0.5, R[:, 1:n + 1], ALU.mult, ALU.subtract, [bT, bR], [bT])
            if post is None:
                stt('dve', dst[:, 0:n], T[:, 0:n], mu[:, j:j + 1], R[:, 1:n + 1], ALU.mult, ALU.add, [bT, bR, b_cC], [b_dst])
            else:
                stt('dve', T[:, 0:n], T[:, 0:n], mu[:, j:j + 1], R[:, 1:n + 1], ALU.mult, ALU.add, [bT, bR, b_cC], [bT])
                act(dst[:, 0:n], T[:, 0:n], post, [bT], [b_dst])

        def phaseC_tile(pc0, n, c0, own0):
            nch = n // 64
            load_shift(T_XW + 0, pc0, n, lx[0], b_lx[0], AF.Tanh)
            load_shift(T_XW + 1, pc0, n, lx[1], b_lx[1], AF.Tanh)
            load_shift(T_XA + 0, pc0, n, lx[2], b_lx[2], AF.Copy)
            load_shift(T_XA + 1, pc0, n, lx[3], b_lx[3], AF.Copy)
            load_shift(T_XG + 0, pc0, n, lx[4], b_lx[4], AF.Sigmoid)
            load_shift(T_XG + 1, pc0, n, lx[5], b_lx[5], AF.Sigmoid)
            for ct in range(8):
                cs_ = slice(ct * P, (ct + 1) * P)
                load_shift(T_R + ct, pc0, n, rr, b_rr)
                load_shift(T_K + ct, pc0, n, kk_, b_k)
                load_shift(T_V + ct, pc0, n, vv_, b_v)
                dma(VSd[cs_, c0 * 64:c0 * 64 + n], vv_[:, 0:n], reads=[b_v])
                for d in range(2):
                    mm(ps[d][:, 0:n], w2s[0:96, d, cs_], lx[d][0:96, 0:n], [b_cC, b_lx[d]], [bps[d]])
                    act(sg[d][:, 0:n], ps[d][:, 0:n], AF.Sigmoid, [bps[d], b_cC], [b_sg[d]], bias=chp[:, ct, d:d + 1])
                    mm(ps[2 + d][:, 0:n], a2s[0:96, d, cs_], lx[2 + d][0:96, 0:n], [b_cC, b_lx[2 + d]], [bps[2 + d]])
                    act(aa[d][:, 0:n], ps[2 + d][:, 0:n], AF.Sigmoid, [bps[2 + d], b_cC], [b_aa[d]], bias=chp[:, ct, 2 + d:3 + d])
                if own0 is not None:
                    mm(ps[4][:, 0:n], g2s[:, 0, cs_], lx[4][:, 0:n], [b_cC, b_lx[4]], [bps[4]], start=True, stop=False)
                    mm(ps[4][:, 0:n], g2s[:, 1, cs_], lx[5][:, 0:n], [b_cC, b_lx[5]], [bps[4]], start=False, stop=True)
                    act(gst[:, 0:n], ps[4][:, 0:n], AF.Copy, [bps[4]], [b_gst])
                    dma(GGd[cs_, own0:own0 + n], gst[:, 0:n], reads=[b_gst])
                ts('dve', kn_[:, 0:n], kk_[:, 0:n], chp[:, ct, 4:5], None, ALU.mult, None, [b_k, b_cC], [b_kn])
                act(t1[:, 0:n], kn_[:, 0:n], AF.Square, [b_kn], [b_t1])
                mm(ps[5][:, 0:n], bonesC, t1[:, 0:n], [b_cC, b_t1], [bps[5]])
                act(t1[:, 0:n], ps[5][:, 0:n], AF.Sqrt, [bps[5]], [b_t1], bias=1e-12)
                rcp(t1[:, 0:n], t1[:, 0:n], [b_t1], [b_t1])
                tt('dve', kn_[:, 0:n], kn_[:, 0:n], t1[:, 0:n], ALU.mult, [b_kn, b_t1], [b_kn])
                for d in range(2):
                    ts('pool', kd[d][:, 0:n], aa[d][:, 0:n], chp[:, ct, 5:6], omka[:, ct:ct + 1], ALU.mult, ALU.add, [b_aa[d], b_cC], [b_kd[d]])
                    tt('pool', kd[d][:, 0:n], kd[d][:, 0:n], kk_[:, 0:n], ALU.mult, [b_kd[d], b_k], [b_kd[d]])
                    tt('pool', bb[:, 0:n], kn_[:, 0:n], aa[d][:, 0:n], ALU.mult, [b_kn, b_aa[d]], [b_bb])
                    S.op('dve', (lambda d: lambda e: e.tensor_tensor_scan(out=csg[:, 0:n], data0=rstm[:, 0:n], data1=sg[d][:, 0:n], initial=0.0,
                                                                           op0=ALU.mult, op1=ALU.add))(d),
                         reads=[b_cC, b_sg[d]], writes=[b_csg])
                    c3 = csg[:, 0:n].rearrange("p (a b) -> p a b", b=64)
                    if d == 0:
                        tt('dve', cpv[:, 0:n], csg[:, 0:n], sg[d][:, 0:n], ALU.subtract, [b_csg, b_sg[d]], [b_cpv])
                        cum = csg; b_cum = b_csg
                        act(gcs[:, 0:nch], c3[:, :, 63], AF.Exp, [b_csg], [b_gcs], scale=-C0)
                    else:
                        act(gcs[:, 0:nch], c3[:, :, 63], AF.Exp, [b_csg], [b_gcs], scale=-C0)
                        stt('dve', cpv[:, 0:n].rearrange("p (a b) -> p a b", b=64), c3, -1.0, c3[:, :, 63:64].to_broadcast([P, nch, 64]),
                            ALU.mult, ALU.add, [b_csg], [b_cpv])
                        tt('dve', csg[:, 0:n], cpv[:, 0:n], sg[d][:, 0:n], ALU.add, [b_cpv, b_sg[d]], [b_csg])
                        cum = csg; b_cum = b_csg
                    dma(GCd[d, cs_, c0:c0 + nch], gcs[:, 0:nch], reads=[b_gcs])
                    act(gm[:, 0:n], cum[:, 0:n], AF.Exp, [b_cum], [b_gm], scale=C0)
                    act(gq[:, 0:n], cum[:, 0:n], AF.Exp, [b_cum], [b_gq], scale=-C0)
                    act(gp[:, 0:n], cpv[:, 0:n], AF.Exp, [b_cpv], [b_gp], scale=-C0)
                    oi = [cC['o'] % 4, (cC['o'] + 1) % 4, (cC['o'] + 2) % 4, (cC['o'] + 3) % 4]; cC['o'] += 4
                    tt('dve', o4[oi[0]][:, 0:n], kd[d][:, 0:n], gm[:, 0:n], ALU.mult, [b_kd[d], b_gm], [b_o4[oi[0]]])
                    tt('pool', o4[oi[1]][:, 0:n], bb[:, 0:n], gm[:, 0:n], ALU.mult, [b_bb, b_gm], [b_o4[oi[1]]])
                    tt('dve', o4[oi[2]][:, 0:n], kn_[:, 0:n], gp[:, 0:n], ALU.mult, [b_kn, b_gp], [b_o4[oi[2]]])
                    tt('pool', o4[oi[3]][:, 0:n], rr[:, 0:n], gq[:, 0:n], ALU.mult, [b_rr, b_gq], [b_o4[oi[3]]])
                    KBv = KBd[d, cs_, c0 * 128:(c0 + nch) * 128].rearrange("p (a b) -> p a b", b=128)
                    KRv = KRd[d, cs_, c0 * 128:(c0 + nch) * 128].rearrange("p (a b) -> p a b", b=128)
                    dma(KBv[:, :, 0:64], o4[oi[0]][:, 0:n].rearrange("p (a b) -> p a b", b=64), reads=[b_o4[oi[0]]])
                    dma(KBv[:, :, 64:128], o4[oi[1]][:, 0:n].rearrange("p (a b) -> p a b", b=64), reads=[b_o4[oi[1]]])
                    dma(KRv[:, :, 0:64], o4[oi[2]][:, 0:n].rearrange("p (a b) -> p a b", b=64), reads=[b_o4[oi[2]]])
                    dma(KRv[:, :, 64:128], o4[oi[3]][:, 0:n].rearrange("p (a b) -> p a b", b=64), reads=[b_o4[oi[3]]])
                if own0 is not None:
                    tt('dve', t1[:, 0:n], kd[0][:, 0:n], kd[1][:, 0:n], ALU.add, [b_kd[0], b_kd[1]], [b_t1])
                    stt('dve', t1[:, 0:n], rr[:, 0:n], chp[:, ct, 6:7], t1[:, 0:n], ALU.mult, ALU.mult, [b_rr, b_cC, b_t1], [b_t1])
                    mm(ps[5][:, 0:n], bonesC, t1[:, 0:n], [b_cC, b_t1], [bps[5]])
                    tt('dve', gst[:, 0:n], ps[5][:, 0:n], vv_[:, 0:n], ALU.mult, [bps[5], b_v], [b_gst])
                    dma(BON[cs_, own0:own0 + n], gst[:, 0:n], reads=[b_gst])

        phaseC_tile(PR_CTX, CTX, 0, None)
        nlt = (debug or {}).get('nlt') or SEQ // 512
        for lt in range(nlt):
            phaseC_tile(PR_LAT + lt * 512, 512, 4 + lt * 8, lt * 512 if lt * 512 < OWN else None)
        S.barrier()
        AF32.release()
    if debug and debug.get('stop') == 'C':
        return finish(nc, S, stack)

    AF32.mark()
    id64 = ident[0:64, 0:64]
    id4 = AF32.alloc(4, 64); maskA = AF32.alloc(2, 320); ones64 = AF32.alloc(64); lnx = AF32.alloc(16, 2); b_cD = Buf()
    for u in range(4):
        cp('dve', id4[0:64, u, :], id64, [b_ident], [b_cD])
    dma(maskA[0:64], maskA_in.rearrange("d p c -> p d c"), writes=[b_cD])
    dma(ones64[0:64], ones64_in, writes=[b_cD]); dma(lnx[0:64], lnx_in.rearrange("p (a b) -> p a b", a=16), writes=[b_cD])
    S0T = AF32.alloc(4, 64); b_S0 = Buf()
    gct = AF32.alloc(4, NCH); b_gct = Buf()
    YT = AF32.alloc(4, OWN); b_YT = Buf()
    GL = 4
    kbL = [AF32.alloc(4, GL * 128) for _ in range(2)]; b_kbL = [Buf(), Buf()]
    krL = [AF32.alloc(4, GL * 128) for _ in range(2)]; b_krL = [Buf(), Buf()]
    vvL = [AF32.alloc(4, GL * 64) for _ in range(2)]; b_vvL = [Buf(), Buf()]
    atb = [AF32.alloc(4, 320) for _ in range(2)]; b_at = [Buf(), Buf()]
    Xb = [AF32.alloc(4, 64) for _ in range(2)]; b_X = [Buf(), Buf()]
    pq = [AF32.alloc(4, 128) for _ in range(2)]; b_pq = [Buf(), Buf()]
    kbt = [AF32.alloc(4, 128) for _ in range(2)]; b_kbt = [Buf(), Buf()]
    vtt = [AF32.alloc(4, 64) for _ in range(2)]; b_vtt = [Buf(), Buf()]
    wm = AF32.alloc(4, 64); b_wm = Buf()
    nsa = AF32.alloc(4, 64); b_nsa = Buf()
    ey = [AF32.alloc(512) for _ in range(2)]; b_ey = [Buf(), Buf()]
    esq = AF32.alloc(512); b_esq = Buf()
    ers = AF32.alloc(512); b_ers = Buf()
    ebo = [AF32.alloc(512) for _ in range(2)]; b_ebo = [Buf(), Buf()]
    egg = [AF32.alloc(512) for _ in range(2)]; b_egg = [Buf(), Buf()]
    eout = [AF32.alloc(512, dt=BF16) for _ in range(2)]; b_eout = [Buf(), Buf()]
    NSTEP = (debug or {}).get('nstep') or 132
    NFWD = min(68, NSTEP)

    def scan_ct(ct):
        def rows(u):
            hh = u % 2
            return slice((2 * ct + hh) * 64, (2 * ct + hh + 1) * 64)
        S.op('dve', lambda e: e.memset(S0T[0:64], 0.0), writes=[b_S0])
        for u in range(4):
            dma(gct[0:64, u, :], GCd[u // 2, rows(u), :], writes=[b_gct])

        def chunk_of(u, i):
            if u < 2:
                return i
            return 3 - i if i < 4 else 135 - i

        def active(u, i):
            return (u >= 2) or (i < NFWD)

        def load_group(g):
            li = g % 2
            for u in range(4):
                if not active(u, 4 * g):
                    continue
                if u < 2:
                    cst = 4 * g
                else:
                    cst = 0 if g == 0 else 132 - 4 * g
                d = u // 2
                dma(kbL[li][0:64, u, :], KBd[d, rows(u), cst * 128:(cst + GL) * 128], writes=[b_kbL[li]])
                dma(krL[li][0:64, u, :], KRd[d, rows(u), cst * 128:(cst + GL) * 128], writes=[b_krL[li]])
                dma(vvL[li][0:64, u, :], VSd[rows(u), cst * 64:(cst + GL) * 64], writes=[b_vvL[li]])

        def views(u, i):
            g = i // 4; li = g % 2
            if u < 2:
                j = i - 4 * g
            else:
                cst = 0 if g == 0 else 132 - 4 * g
                j = chunk_of(u, i) - cst
            KB = kbL[li][0:64, u, j * 128:(j + 1) * 128]
            KR = krL[li][0:64, u, j * 128:(j + 1) * 128]
            V = vvL[li][0:64, u, j * 64:(j + 1) * 64]
            return KB, KR, V, [b_kbL[li], b_krL[li], b_vvL[li]]

        def prep(i):
            bi = i % 2
            AT = atb[bi]; bAT = b_at[bi]; X = Xb[bi]; bX = b_X[bi]
            us = [u for u in range(4) if active(u, i)]
            u0, u1 = us[0], us[-1] + 1
            for u in us:
                KB, KR, V, bl = views(u, i)
                pa = ps[u % 2]; bpa = bps[u % 2]
                mm(pa[0:64, 0:128], KB[:, 0:64], KR[:, 0:128], bl, [bpa])
                mm(pa[0:64, 128:256], KB[:, 64:128], KR[:, 0:128], bl, [bpa])
                mm(pa[0:64, 256:320], KR[:, 0:64], KB[:, 64:128], bl, [bpa])
                tt('dve', AT[0:64, u, :], pa[0:64, 0:320], maskA[0:64, u // 2, :], ALU.mult, [bpa, b_cD], [bAT])
                tr(ps[4][0:64, u * 128:u * 128 + 64], KB[:, 0:64], id64, bl + [b_ident], [bps[4]])
                tr(ps[4][0:64, u * 128 + 64:u * 128 + 128], KB[:, 64:128], id64, bl + [b_ident], [bps[4]])
                tr(ps[5][0:64, u * 64:(u + 1) * 64], V, id64, bl + [b_ident], [bps[5]])
            cp('act', kbt[bi][0:64, u0:u1, :], ps[4][0:64, u0 * 128:u1 * 128].rearrange("p (a b) -> p a b", b=128), [bps[4]], [b_kbt[bi]])
            cp('act', vtt[bi][0:64, u0:u1, :], ps[5][0:64, u0 * 64:u1 * 64].rearrange("p (a b) -> p a b", b=64), [bps[5]], [b_vtt[bi]])
            tt('dve', X[0:64, u0:u1, :], id4[0:64, u0:u1, :], AT[0:64, u0:u1, 128:192], ALU.subtract, [b_cD, bAT], [bX])
            yield
            for it in range(5):
                pj = it % 2
                for u in us:
                    if it == 0:
                        Pm = AT[0:64, u, 128:192]; Qm = AT[0:64, u, 256:320]; bsrc = bAT
                    else:
                        Pm = pq[1 - pj][0:64, u, 0:64]; Qm = pq[1 - pj][0:64, u, 64:128]; bsrc = b_pq[1 - pj]
                    if it < 4:
                        mm(ps[2][0:64, u * 128:u * 128 + 64], Qm, Pm, [bsrc], [bps[2]])
                    mm(ps[2][0:64, u * 128 + 64:u * 128 + 128], Pm, Qm, [bsrc], [bps[2]])
                cp('act', pq[pj][0:64, u0:u1, :], ps[2][0:64, u0 * 128:u1 * 128].rearrange("p (a b) -> p a b", b=128), [bps[2]], [b_pq[pj]])
                yield
                for u in us:
                    mm(ps[3][0:64, u * 64:(u + 1) * 64], pq[pj][0:64, u, 64:128], X[0:64, u, :], [b_pq[pj], bX], [bps[3]])
                tt('dve', X[0:64, u0:u1, :], X[0:64, u0:u1, :], ps[3][0:64, u0 * 64:u1 * 64].rearrange("p (a b) -> p a b", b=64), ALU.add,
                   [bX, bps[3]], [bX])
                yield

        def chain(i):
            bi = i % 2
            AT = atb[bi]; bAT = b_at[bi]; X = Xb[bi]; bX = b_X[bi]
            KT_ = kbt[bi]; bKT = b_kbt[bi]; VT = vtt[bi]; bVT = b_vtt[bi]
            us = [u for u in range(4) if active(u, i)]
            u0, u1 = us[0], us[-1] + 1
            for u in us:
                KB, KR, V, bl = views(u, i)
                mm(ps[6][0:64, u * 64:(u + 1) * 64], KR[:, 0:64], S0T[0:64, u, :], bl + [b_S0], [bps[6]], start=True, stop=False)
                mm(ps[6][0:64, u * 64:(u + 1) * 64], AT[0:64, u, 0:64], VT[0:64, u, :], [bAT, bVT], [bps[6]], start=False, stop=True)
            cp('act', wm[0:64, u0:u1, :], ps[6][0:64, u0 * 64:u1 * 64].rearrange("p (a b) -> p a b", b=64), [bps[6]], [b_wm])
            yield
            for u in us:
                mm(ps[7][0:64, u * 64:(u + 1) * 64], X[0:64, u, :], wm[0:64, u, :], [bX, b_wm], [bps[7]])
            ts('dve', nsa[0:64, u0:u1, :], ps[7][0:64, u0 * 64:u1 * 64].rearrange("p (a b) -> p a b", b=64), -1.0, None, ALU.mult, None,
               [bps[7]], [b_nsa])
            yield
            need_y = [u for u in us if 4 <= chunk_of(u, i) < 4 + OWN // 64]
            for u in need_y:
                KB, KR, V, bl = views(u, i)
                mm(ps[2][0:64, u * 64:(u + 1) * 64], S0T[0:64, u, :], KR[:, 64:128], bl + [b_S0], [bps[2]], start=True, stop=False)
                mm(ps[2][0:64, u * 64:(u + 1) * 64], VT[0:64, u, :], AT[0:64, u, 64:128], [bVT, bAT], [bps[2]], start=False, stop=False)
                mm(ps[2][0:64, u * 64:(u + 1) * 64], nsa[0:64, u, :], AT[0:64, u, 192:256], [b_nsa, bAT], [bps[2]], start=False, stop=True)
            for dd in range(2):
                uu = [u for u in need_y if u // 2 == dd]
                if not uu:
                    continue
                t0 = (chunk_of(uu[0], i) - 4) * 64
                cp('act', YT[0:64, uu[0]:uu[-1] + 1, t0:t0 + 64],
                   ps[2][0:64, uu[0] * 64:(uu[-1] + 1) * 64].rearrange("p (a b) -> p a b", b=64), [bps[2]], [b_YT])
            for u in us:
                mm(ps[3][0:64, u * 64:(u + 1) * 64], KT_[0:64, u, 0:64], VT[0:64, u, :], [bKT, bVT], [bps[3]], start=True, stop=False)
                mm(ps[3][0:64, u * 64:(u + 1) * 64], KT_[0:64, u, 64:128], nsa[0:64, u, :], [bKT, b_nsa], [bps[3]], start=False, stop=False)
                mm(ps[3][0:64, u * 64:(u + 1) * 64], id64, S0T[0:64, u, :], [b_ident, b_S0], [bps[3]], start=False, stop=True)
            for dd in range(2):
                uu = [u for u in us if u // 2 == dd]
                if not uu:
                    continue
                c = chunk_of(uu[0], i)
                a, b2 = uu[0], uu[-1] + 1
                tt('dve', S0T[0:64, a:b2, :], ps[3][0:64, a * 64:b2 * 64].rearrange("p (a b) -> p a b", b=64),
                   gct[0:64, a:b2, c:c + 1].to_broadcast([64, b2 - a, 64]), ALU.mult, [bps[3], b_gct], [b_S0])

        def interleave(gens):
            gens = list(gens)
            while gens:
                for g_ in list(gens):
                    try:
                        next(g_)
                    except StopIteration:
                        gens.remove(g_)

        load_group(0)
        interleave([prep(0)])
        for i in range(NSTEP):
            if i + 1 < NSTEP:
                if (i + 1) % 4 == 0:
                    load_group((i + 1) // 4)
                interleave([prep(i + 1), chain(i)])
            else:
                interleave([chain(i)])
        for hh in range(2):
            head = 2 * ct + hh
            hr = slice(head * 64, (head + 1) * 64)
            for c8 in range(OWN // 512):
                cs2 = slice(c8 * 512, (c8 + 1) * 512)
                i2 = c8 % 2
                Y = ey[i2]; bY = b_ey[i2]
                dma(ebo[i2][0:64], BON[hr, cs2], writes=[b_ebo[i2]])
                dma(egg[i2][0:64], GGd[hr, cs2], writes=[b_egg[i2]])
                tt('dve', Y[0:64], YT[0:64, hh, cs2], YT[0:64, 2 + hh, cs2], ALU.add, [b_YT], [bY])
                mm(ps[0][0:64, :], ones64[0:64], Y[0:64], [b_cD, bY], [bps[0]])
                tt('dve', Y[0:64], Y[0:64], ps[0][0:64, :], ALU.subtract, [bY, bps[0]], [bY])
                act(esq[0:64], Y[0:64], AF.Square, [bY], [b_esq])
                mm(ps[1][0:64, :], ones64[0:64], esq[0:64], [b_cD, b_esq], [bps[1]])
                act(ers[0:64], ps[1][0:64, :], AF.Sqrt, [bps[1]], [b_ers], bias=64e-5)
                rcp(ers[0:64], ers[0:64], [b_ers], [b_ers])
                tt('dve', Y[0:64], Y[0:64], ers[0:64], ALU.mult, [bY, b_ers], [bY])
                ts('dve', Y[0:64], Y[0:64], lnx[0:64, head, 0:1], lnx[0:64, head, 1:2], ALU.mult, ALU.add, [bY, b_cD], [bY])
                tt('pool', Y[0:64], Y[0:64], ebo[i2][0:64], ALU.add, [bY, b_ebo[i2]], [bY])
                tt('dve', eout[i2][0:64], Y[0:64], egg[i2][0:64], ALU.mult, [bY, b_egg[i2]], [b_eout[i2]])
                dma(YR[hr, cs2], eout[i2][0:64], reads=[b_eout[i2]])

    nct = (debug or {}).get('nct') or 8
    for ct in range(nct):
        scan_ct(ct)
    S.barrier()
    AF32.release()
    if debug and debug.get('stop') == 'E':
        return finish(nc, S, stack)

    AF32.mark()
    wpab = AF32.alloc(8, D, dt=BF16); wpbb = AF32.alloc(8, D, dt=BF16); b_wp = Buf()
    wst = [AF32.alloc(D) for _ in range(2)]; b_wst = [Buf(), Buf()]
    k_ = 0
    for (src_w, dstw) in ((wpa_in, wpab), (wpb_in, wpbb)):
        for kc in range(8):
            i = k_ % 2; k_ += 1
            dma(wst[i], src_w[kc * P:(kc + 1) * P, :], writes=[b_wst[i]])
            cp('pool' if kc % 2 else 'dve', dstw[:, kc, :], wst[i], [b_wst[i]], [b_wp])
    yrT = [AF32.alloc(8, 512, dt=BF16) for _ in range(2)]; b_yrT = [Buf(), Buf()]
    ydT = [AF32.alloc(8, 512, dt=BF16) for _ in range(2)]; b_ydT = [Buf(), Buf()]
    g0t = [AF32.alloc(512) for _ in range(2)]; g1t = [AF32.alloc(512) for _ in range(2)]
    b_g0t = [Buf(), Buf()]; b_g1t = [Buf(), Buf()]
    ta = [AF32.alloc(512) for _ in range(2)]; tb_ = [AF32.alloc(512) for _ in range(2)]
    b_ta = [Buf(), Buf()]; b_tb = [Buf(), Buf()]
    mxo = [AF32.alloc(512, dt=BF16) for _ in range(2)]; b_mxo = [Buf(), Buf()]
    YRv = YR.rearrange("(k p) t -> p k t", p=P)
    YDv = YD.rearrange("h p t -> p h t")
    NCK = (debug or {}).get('nck') or OWN // 512
    cg = 0
    for ck in range(NCK):
        cs3 = slice(ck * 512, (ck + 1) * 512)
        ci = ck % 2
        dma(yrT[ci], YRv[:, :, cs3], writes=[b_yrT[ci]])
        dma(ydT[ci], YDv[:, :, cs3], writes=[b_ydT[ci]])
        for ft in range(16):
            i = cg % 2; cg += 1
            dma(g0t[i], GT[ft, :, cs3], writes=[b_g0t[i]])
            dma(g1t[i], GT[16 + ft, :, cs3], writes=[b_g1t[i]])
            pa_ = ps[i]; pb_ = ps[2 + i]
            for kc in range(8):
                mm(pa_[:, :], wpab[:, kc, ft * P:(ft + 1) * P], yrT[ci][:, kc, :], [b_wp, b_yrT[ci]], [bps[i]], start=(kc == 0), stop=(kc == 7))
            for kc in range(8):
                mm(pb_[:, :], wpbb[:, kc, ft * P:(ft + 1) * P], ydT[ci][:, kc, :], [b_wp, b_ydT[ci]], [bps[2 + i]], start=(kc == 0), stop=(kc == 7))
            tt('dve', ta[i], pa_[:, :], g0t[i], ALU.mult, [bps[i], b_g0t[i]], [b_ta[i]])
            tt('dve', tb_[i], pb_[:, :], g1t[i], ALU.mult, [bps[2 + i], b_g1t[i]], [b_tb[i]])
            tt('pool', mxo[i], ta[i], tb_[i], ALU.add, [b_ta[i], b_tb[i]], [b_mxo[i]])
            dma(MX[ft, :, cs3], mxo[i], reads=[b_mxo[i]])
    S.barrier()
    AF32.release()

    AF32.mark()
    woutb = AF32.alloc(16, D, dt=BF16); b_wo = Buf()
    wst = [AF32.alloc(D) for _ in range(2)]; b_wst = [Buf(), Buf()]
    for ft in range(16):
        i = ft % 2
        dma(wst[i], wout_in[ft * P:(ft + 1) * P, :], writes=[b_wst[i]])
        cp('pool' if ft % 2 else 'dve', woutb[:, ft, :], wst[i], [b_wst[i]], [b_wo])
    gt1b = AF32.alloc(D); A2b = AF32.alloc(D); B2b = AF32.alloc(D); wr = AF32.alloc(KC, 36); rbias = AF32.alloc(36); b_cG = Buf()
    dma(gt1b, MODS[8:9, :].partition_broadcast(P), reads=[b_MODS], writes=[b_cG])
    dma(A2b, MODS[4:5, :].partition_broadcast(P), reads=[b_MODS], writes=[b_cG])
    dma(B2b, MODS[6:7, :].partition_broadcast(P), reads=[b_MODS], writes=[b_cG])
    dma(wr, wr_in.rearrange("p (a b) -> p a b", a=KC), writes=[b_cG])
    dma(rbias, rbias_in.partition_broadcast(P), writes=[b_cG])
    mxT = [AF32.alloc(16, P, dt=BF16) for _ in range(2)]; b_mxT = [Buf(), Buf()]
    xg_ = [AF32.alloc(D) for _ in range(2)]; b_xg = [Buf(), Buf()]
    xn_ = [AF32.alloc(D) for _ in range(2)]; b_xn = [Buf(), Buf()]
    h2 = AF32.alloc(D); b_h2 = Buf()
    junk2 = AF32.alloc(D, dt=BF16); b_junk2 = Buf()
    st2 = [AF32.alloc(4) for _ in range(2)]; b_st2 = [Buf(), Buf()]
    h2T32 = AF32.alloc(KC, P); b_h2T32 = Buf()
    h2Tb = [AF32.alloc(KC, P, dt=BF16) for _ in range(2)]; b_h2Tb = [Buf(), Buf()]
    rt = [AF32.alloc(160) for _ in range(2)]; b_rt = [Buf(), Buf()]
    gmo = [AF32.alloc(P) for _ in range(2)]; b_gmo = [Buf(), Buf()]
    MXv = MX.rearrange("f p t -> p f t")
    H2Tv = H2T.rearrange("k p t -> p k t")
    NTT = (debug or {}).get('ntt') or OWN // P
    for t_ in range(NTT):
        i = t_ % 2
        ts_ = slice(t_ * P, (t_ + 1) * P)
        dma(mxT[i], MXv[:, :, ts_], writes=[b_mxT[i]])
        dma(xg_[i], x_loc[ts_, :], writes=[b_xg[i]])
        XNt = xn_[i]; bXN = b_xn[i]
        for nt in range(4):
            ns = slice(nt * 512, (nt + 1) * 512)
            for ft in range(16):
                mm(ps[nt][:, :], mxT[i][:, ft, :], woutb[:, ft, ns], [b_mxT[i], b_wo], [bps[nt]], start=(ft == 0), stop=(ft == 15))
            tt('dve', XNt[:, ns], ps[nt][:, :], gt1b[:, ns], ALU.mult, [bps[nt], b_cG], [bXN])
        tt('pool', XNt, XNt, xg_[i], ALU.add, [bXN, b_xg[i]], [bXN])
        dma(XN[ts_, :], XNt, reads=[bXN])
        sq2 = st2[i]; bsq = b_st2[i]
        act(junk2, XNt, AF.Square, [bXN], [b_junk2, bsq], accum_out=sq2[:, 0:1])
        act(sq2[:, 1:2], sq2[:, 0:1], AF.Sqrt, [bsq], [bsq], scale=1.0 / D, bias=1e-6)
        rcp(sq2[:, 1:2], sq2[:, 1:2], [bsq], [bsq])
        stt('dve', h2, XNt, sq2[:, 1:2], A2b, ALU.mult, ALU.mult, [bXN, bsq, b_cG], [b_h2])
        tt('pool', h2, h2, B2b, ALU.add, [b_h2, b_cG], [b_h2])
        for q4 in range(4):
            pb_i = 4 + q4 % 2
            for q in range(4):
                kc = q4 * 4 + q
                tr(ps[pb_i][:, q * P:(q + 1) * P], h2[:, kc * P:(kc + 1) * P], ident, [b_h2, b_ident], [bps[pb_i]])
            cp('act' if q4 % 2 == 0 else 'dve', h2T32[:, q4 * 4:q4 * 4 + 4, :], ps[pb_i][:, :].rearrange("p (a b) -> p a b", a=4), [bps[pb_i]], [b_h2T32])
        cp('pool', h2Tb[i], h2T32, [b_h2T32], [b_h2Tb[i]])
        dma(H2Tv[:, :, ts_], h2Tb[i], reads=[b_h2Tb[i]])
        for kc in range(KC):
            mm(ps[6][:, 0:36], h2T32[:, kc, :], wr[:, kc, :], [b_h2T32, b_cG], [bps[6]], start=(kc == 0), stop=(kc == KC - 1))
        R = rt[i]; bR = b_rt[i]
        L = R[:, 0:36]
        tt('dve', L, ps[6][:, 0:36], rbias, ALU.add, [bps[6], b_cG], [bR])
        sc_ = R[:, 136:160]
        S.op('dve', (lambda R, sc_: lambda e: e.tensor_reduce(out=sc_[:, 0:1], in_=R[:, 0:4], axis=AX.X, op=ALU.max))(R, sc_), reads=[bR], writes=[bR])
        ts('dve', R[:, 36:40], R[:, 0:4], sc_[:, 0:1], None, ALU.is_equal, None, [bR], [bR])
        ts('dve', sc_[:, 1:2], sc_[:, 0:1], -1.0, None, ALU.mult, None, [bR], [bR])
        act(R[:, 104:108], R[:, 0:4], AF.Exp, [bR], [bR], bias=sc_[:, 1:2], accum_out=sc_[:, 2:3])
        rcp(sc_[:, 3:4], sc_[:, 2:3], [bR], [bR])
        ts('dve', R[:, 36:40], R[:, 36:40], 1e30, -1e30, ALU.mult, ALU.add, [bR], [bR])
        tt('dve', R[:, 40:72].rearrange("p (a b) -> p a b", a=4), R[:, 4:36].rearrange("p (a b) -> p a b", a=4),
           R[:, 36:40].rearrange("p (a b) -> p a b", b=1).to_broadcast([P, 4, 8]), ALU.add, [bR], [bR])
        S.op('dve', (lambda R, sc_: lambda e: e.tensor_reduce(out=sc_[:, 4:5], in_=R[:, 40:72], axis=AX.X, op=ALU.max))(R, sc_), reads=[bR], writes=[bR])
        ts('dve', R[:, 72:104], R[:, 40:72], sc_[:, 4:5], None, ALU.is_equal, None, [bR], [bR])
        stt('dve', R[:, 104:136], R[:, 72:104], -1e30, R[:, 40:72], ALU.mult, ALU.add, [bR], [bR])
        S.op('dve', (lambda R, sc_: lambda e: e.tensor_reduce(out=sc_[:, 5:6], in_=R[:, 104:136], axis=AX.X, op=ALU.max))(R, sc_), reads=[bR], writes=[bR])
        ts('dve', R[:, 104:136], R[:, 104:136], sc_[:, 5:6], None, ALU.is_equal, None, [bR], [bR])
        tt('dve', sc_[:, 6:7], sc_[:, 4:5], sc_[:, 5:6], ALU.subtract, [bR], [bR])
        act(sc_[:, 7:8], sc_[:, 6:7], AF.Sigmoid, [bR], [bR])
        tt('dve', sc_[:, 8:9], sc_[:, 7:8], sc_[:, 3:4], ALU.mult, [bR], [bR])
        tt('dve', sc_[:, 9:10], sc_[:, 3:4], sc_[:, 8:9], ALU.subtract, [bR], [bR])
        ts('dve', R[:, 72:104], R[:, 72:104], sc_[:, 8:9], None, ALU.mult, None, [bR], [bR])
        stt('dve', R[:, 40:72], R[:, 104:136], sc_[:, 9:10], R[:, 72:104], ALU.mult, ALU.add, [bR], [bR])
        tr(ps[7][0:32, 0:P], R[:, 40:72], ident, [bR, b_ident], [bps[7]])
        cp('act', gmo[i][0:32, :], ps[7][0:32, 0:P], [bps[7]], [b_gmo[i]])
        dma(GMT[:, ts_], gmo[i][0:32, :], reads=[b_gmo[i]])
    S.barrier()
    AF32.release()
    if debug and debug.get('stop') == 'G':
        return finish(nc, S, stack)

    AF32.mark()
    wfs = [AF32.alloc(KC * DEXP) for _ in range(2)]; b_wfs = [Buf(), Buf()]
    wbs = [AF32.alloc(KC * DEXP, dt=BF16) for _ in range(2)]; b_wbs = [Buf(), Buf()]
    NE = (debug or {}).get('nexp') or NEXP
    k_ = 0
    for e_ in range(NE):
        for (srcw, dstw) in ((w1T_in, W1B), (w3T_in, W3B), (w2T_in, W2B)):
            i = k_ % 2
            dma(wfs[i], srcw[e_], writes=[b_wfs[i]])
            half = KC * DEXP // 2
            cp('dve', wbs[i][:, 0:half], wfs[i][:, 0:half], [b_wfs[i]], [b_wbs[i]])
            cp('pool' if k_ % 2 else 'act', wbs[i][:, half:], wfs[i][:, half:], [b_wfs[i]], [b_wbs[i]])
            dma(dstw[e_], wbs[i], reads=[b_wbs[i]])
            k_ += 1
    S.barrier()
    AF32.release()

    AF32.mark()
    ST = 1024
    h2s = AF32.alloc(KC, ST, dt=BF16); b_h2s = Buf()
    yacc = AF32.alloc(ST // P, D); b_yacc = Buf()
    w1b = AF32.alloc(KC, DEXP, dt=BF16); w3b = AF32.alloc(KC, DEXP, dt=BF16); w2b = AF32.alloc(4, D, dt=BF16)
    b_w1b = Buf(); b_w3b = Buf(); b_w2b = Buf()
    uT = AF32.alloc(4, ST, dt=BF16); b_uT = Buf()
    gb = [AF32.alloc(ST) for _ in range(2)]; b_gb = [Buf(), Buf()]
    s1 = [AF32.alloc(512) for _ in range(2)]; b_s1 = [Buf(), Buf()]
    t3 = [AF32.alloc(512) for _ in range(2)]; b_t3 = [Buf(), Buf()]
    gt2b = AF32.alloc(D); b_gt2 = Buf()
    xno = AF32.alloc(D); b_xno = Buf()
    dma(gt2b, MODS[10:11, :].partition_broadcast(P), reads=[b_MODS], writes=[b_gt2])
    b_out = Buf()
    NST = (debug or {}).get('nst') or OWN // ST
    kk2 = 0
    for st_ in range(NST):
        tsl = slice(st_ * ST, (st_ + 1) * ST)
        dma(h2s, H2Tv[:, :, tsl], writes=[b_h2s])
        for e_ in range(NE):
            gi = e_ % 2
            dma(gb[gi], GMT[e_:e_ + 1, tsl].partition_broadcast(P), writes=[b_gb[gi]])
            dma(w1b, W1B[e_].rearrange("p (a b) -> p a b", a=KC), writes=[b_w1b])
            dma(w3b, W3B[e_].rearrange("p (a b) -> p a b", a=KC), writes=[b_w3b])
            dma(w2b, W2B[e_].rearrange("p (a b) -> p a b", a=4), writes=[b_w2b])
            for dt_ in range(4):
                for tch in range(ST // 512):
                    i = kk2 % 2; kk2 += 1
                    cs4 = slice(tch * 512, (tch + 1) * 512)
                    for kc in range(KC):
                        mm(ps[i][:, :], w1b[:, kc, dt_ * P:(dt_ + 1) * P], h2s[:, kc, cs4], [b_w1b, b_h2s], [bps[i]], start=(kc == 0), stop=(kc == KC - 1))
                    for kc in range(KC):
                        mm(ps[2 + i][:, :], w3b[:, kc, dt_ * P:(dt_ + 1) * P], h2s[:, kc, cs4], [b_w3b, b_h2s], [bps[2 + i]], start=(kc == 0), stop=(kc == KC - 1))
                    act(s1[i], ps[i][:, :], AF.Silu, [bps[i]], [b_s1[i]])
                    tt('dve', t3[i], ps[2 + i][:, :], gb[gi][:, cs4], ALU.mult, [bps[2 + i], b_gb[gi]], [b_t3[i]])
                    tt('pool', uT[:, dt_, cs4], s1[i], t3[i], ALU.mult, [b_s1[i], b_t3[i]], [b_uT])
            for tt_ in range(ST // P):
                for nt in range(4):
                    pi = 4 + (tt_ * 4 + nt) % 4
                    ns = slice(nt * 512, (nt + 1) * 512)
                    for dt_ in range(4):
                        mm(ps[pi][:, :], uT[:, dt_, tt_ * P:(tt_ + 1) * P], w2b[:, dt_, ns], [b_uT, b_w2b], [bps[pi]], start=(dt_ == 0), stop=(dt_ == 3))
                    if e_ == 0:
                        cp('dve', yacc[:, tt_, ns], ps[pi][:, :], [bps[pi]], [b_yacc])
                    else:
                        tt('dve', yacc[:, tt_, ns], yacc[:, tt_, ns], ps[pi][:, :], ALU.add, [b_yacc, bps[pi]], [b_yacc])
        for tt_ in range(ST // P):
            r0 = st_ * ST + tt_ * P
            dma(xno, XN[r0:r0 + P, :], writes=[b_xno])
            tt('pool', yacc[:, tt_, :], yacc[:, tt_, :], gt2b, ALU.mult, [b_yacc, b_gt2], [b_yacc])
            tt('dve', xno, xno, yacc[:, tt_, :], ALU.add, [b_xno, b_yacc], [b_xno])
            dma(out[r0:r0 + P, :], xno, reads=[b_xno])
    AF32.release()

    return finish(nc, S, stack)


def finish(nc, S, stack):
    S.barrier()
    S.emit()
    return nc, stack


def win_layout(w_in, h):
    C = 1024
    cols = []
    cols.append(w_in[:, 0:3 * C])
    o = 3 * C
    xw = [w_in[:, o:o + 96], w_in[:, o + 96:o + 192]]; o += 192
    xa = [w_in[:, o:o + 96], w_in[:, o + 96:o + 192]]; o += 192
    xg = w_in[:, o:o + 256]; o += 256
    z32 = np.zeros((D, 32), np.float32)
    order = (0, 1) if h == 0 else (1, 0)
    for d in order:
        cols += [xw[d], z32]
    for d in order:
        cols += [xa[d], z32]
    cols.append(xg)
    q = w_in[:, o:o + 1024]; o += 1024
    k = w_in[:, o:o + 1024]; o += 1024
    v = w_in[:, o:o + 1024]; o += 1024
    g = w_in[:, o:o + 4096]
    cols += [q, k, g]
    W = np.concatenate(cols, axis=1)
    assert W.shape[1] == N_FT * P, W.shape
    winT = W.reshape(KC, P, N_FT, P).transpose(2, 1, 0, 3).reshape(N_FT, P, KC * P)
    winV = v.reshape(KC, P, 8, P).transpose(2, 1, 0, 3).reshape(8, P, KC * P)
    return np.ascontiguousarray(winT), np.ascontiguousarray(winV)


def make_inputs(inp, core):
    b, h = core // 2, core % 2
    x = inp['x'][b]; ctx = inp['ctx'][b]
    if h == 1:
        x = x[::-1]; ctx = ctx[::-1]
    m = {}
    m['x_loc'] = np.ascontiguousarray(x)
    m['ctx_loc'] = np.ascontiguousarray(ctx)
    cc = np.stack([inp['c'][b], inp['c_ctx']], axis=-1)
    m['cT'] = np.ascontiguousarray(cc.reshape(KC, P, 2).transpose(1, 0, 2).reshape(P, KC * 2))
    return m


def shared_inputs(inp, h, reuse=None):
    m = {}
    aw = inp['ada_w'][0]
    if reuse is None:
        m['adaT'] = np.ascontiguousarray(aw.reshape(KC, P, 24, 512).transpose(2, 1, 0, 3).reshape(24, P, KC * 512))
    else:
        m['adaT'] = reuse['adaT']
    m['adab'] = np.ascontiguousarray(inp['ada_b'][0][None, :])
    m['n1g'] = np.ascontiguousarray(inp['norm1_g'])
    m['n2g'] = np.ascontiguousarray(inp['norm2_g'])
    m['winT'], m['winV'] = win_layout(inp['w_in'][0], h)
    m['ident'] = np.eye(P, dtype=np.float32)
    bo = np.zeros((P, P), np.float32); bo[:64, :64] = 1; bo[64:, 64:] = 1
    m['bones'] = bo
    rm = np.zeros((P, P), np.float32)
    for mp in range(P):
        if (mp % 32) < 16:
            rm[mp + 16, mp] = -1.0
        else:
            rm[mp - 16, mp] = 1.0
    m['rotm'] = rm
    t = np.arange(SEQ)
    if h == 1:
        t = t[::-1]
    rows = (t // 64).astype(np.float32); colsp = (t % 64).astype(np.float32)
    half = 32
    inv_freq = (10000.0 ** (-np.arange(0, half, 2, dtype=np.float32) / half)).astype(np.float32)
    cosT = np.zeros((P, SEQ), np.float32); sinT = np.zeros((P, SEQ), np.float32)
    for p in range(P):
        d = p % 64
        pos = rows if d < 32 else colsp
        ang = (pos * inv_freq[d % 16]).astype(np.float32)
        cosT[p] = np.cos(ang); sinT[p] = np.sin(ang)
    m['cosT'] = cosT; m['sinT'] = sinT
    m['qkg'] = np.ascontiguousarray(np.stack([np.tile(inp['qn_g'][0], 2), np.tile(inp['kn_g'][0], 2)], axis=1))
    m['dlam'] = np.ascontiguousarray(inp['diff_lambda'][0].reshape(1, 256))
    m['subg'] = np.ascontiguousarray(inp['subln_g'])
    mkk = np.zeros((P, 2), np.float32); mkk[:64, 0] = 1; mkk[64:, 1] = 1
    m['mk'] = mkk
    order = (0, 1) if h == 0 else (1, 0)
    smu = inp['shift_mu'][0]
    mu = np.zeros((P, N_RW), np.float32)
    for j in range(24):
        mu[:, j] = smu[j * P:(j + 1) * P]
    for i, d in enumerate(order):
        mu[:96, T_XW + i] = smu[3072 + d * 96:3072 + (d + 1) * 96]
        mu[:96, T_XA + i] = smu[3072 + 192 + d * 96:3072 + 192 + (d + 1) * 96]
    for t in range(2):
        mu[:, T_XG + t] = smu[3072 + 384 + t * P:3072 + 384 + (t + 1) * P]
    m['mu'] = mu
    w2s = np.zeros((2, P, 1024), np.float32); a2s = np.zeros((2, P, 1024), np.float32)
    for i, d in enumerate(order):
        w2s[i, :96] = inp['rwkv_w2'][0, d]; a2s[i, :96] = inp['rwkv_a2'][0, d]
    m['w2s'] = w2s; m['a2s'] = a2s
    m['g2s'] = np.ascontiguousarray(inp['rwkv_g2'][0].reshape(2, P, 1024).transpose(1, 0, 2).reshape(P, 2048))
    chp = np.zeros((P, 8, 8), np.float32)
    def cl(v):
        return v.reshape(8, P).T
    chp[:, :, 0] = cl(inp['rwkv_w0'][0, order[0]]); chp[:, :, 1] = cl(inp['rwkv_w0'][0, order[1]])
    chp[:, :, 2] = cl(inp['rwkv_a0'][0, order[0]]); chp[:, :, 3] = cl(inp['rwkv_a0'][0, order[1]])
    chp[:, :, 4] = cl(inp['rwkv_k_k'][0]); chp[:, :, 5] = cl(inp['rwkv_k_a'][0]); chp[:, :, 6] = cl(inp['rwkv_r_k'][0].reshape(-1))
    m['chp'] = chp.reshape(P, 64)
    lnx = np.zeros((64, 16, 2), np.float32)
    lnx[:, :, 0] = inp['rwkv_lnx_g'][0].reshape(16, 64).T; lnx[:, :, 1] = inp['rwkv_lnx_b'][0].reshape(16, 64).T
    m['lnx'] = lnx.reshape(64, 32)
    ii = np.arange(64)
    su = (ii[:, None] < ii[None, :]).astype(np.float32); iu = (ii[:, None] <= ii[None, :]).astype(np.float32)
    mA = np.zeros((2, 64, 320), np.float32)
    mA[0] = np.concatenate([su, iu, su, iu, su.T], axis=1)
    mA[1] = np.concatenate([su.T, iu.T, su.T, iu.T, su], axis=1)
    m['maskA'] = mA
    rst = np.ones((P, 512), np.float32); rst[:, ::64] = 0
    m['rst'] = rst
    m['ones64'] = np.full((64, 64), 1.0 / 64, np.float32)
    m['wpa'] = np.ascontiguousarray(inp['w_pa'][0]); m['wpb'] = np.ascontiguousarray(inp['w_pb'][0])
    m['wout'] = np.ascontiguousarray(inp['w_out'][0])
    wrr = np.concatenate([inp['router_g_w'][0], inp['router_e_w'][0]], axis=1)
    m['wr'] = np.ascontiguousarray(wrr.reshape(KC, P, 36).transpose(1, 0, 2).reshape(P, KC * 36))
    m['rbias'] = np.ascontiguousarray(np.concatenate([inp['router_g_b'][0], inp['router_e_b'][0]])[None, :])
    if reuse is None:
        m['w1T'] = np.ascontiguousarray(inp['exp_w1'][0].reshape(NEXP, KC, P, DEXP).transpose(0, 2, 1, 3).reshape(NEXP, P, KC * DEXP))
        m['w3T'] = np.ascontiguousarray(inp['exp_w3'][0].reshape(NEXP, KC, P, DEXP).transpose(0, 2, 1, 3).reshape(NEXP, P, KC * DEXP))
        m['w2T'] = np.ascontiguousarray(inp['exp_w2'][0].reshape(NEXP, 4, P, D).transpose(0, 2, 1, 3).reshape(NEXP, P, 4 * D))
    else:
        m['w1T'] = reuse['w1T']; m['w3T'] = reuse['w3T']; m['w2T'] = reuse['w2T']
    return m


def kernel(**inp):
    inp = {k: np.asarray(v) for k, v in inp.items()}
    nc, stack = build()
    sh0 = shared_inputs(inp, 0)
    sh = [sh0, shared_inputs(inp, 1, reuse=sh0)]
    in_maps = []
    for core in range(8):
        m = dict(sh[core % 2])
        m.update(make_inputs(inp, core))
        in_maps.append(m)
    res = run_bass_kernel_spmd(nc, in_maps, core_ids=list(range(8)))
    outp = np.zeros((4, SEQ, D), np.float32)
    for core in range(8):
        b, h = core // 2, core % 2
        o = res.results[core]["out"]
        if h == 0:
            outp[b, :OWN] = o
        else:
            outp[b, OWN:] = o[::-1]
    return outp
```
